# Optimizing a Trainium2 kernel written in Bass

```python
import math
import jax
import jax.numpy as jnp
from jax import lax
import numpy as np

D_MODEL = 2048
BATCH = 2
SEQ = 8192
DEPTH = 4

HEAD_DIM = 64
EPS = 1e-6
NA_HEADS = 8
NA_KH = 8
NA_KW = 16
GRID_W = 64
WA_Q_HEADS = 8
WA_KV_HEADS = 2
WINDOW = 128
WA_BLOCK = 128
ROPE_THETA = 500000.0
ROPE_DIM = HEAD_DIM // 4
SSM_HEADS = 8
SSM_HEAD_DIM = 64
SSM_GROUPS = 2
SSM_STATE = 128
SSM_CONV = 5
SSM_CHUNK = 128
CONF_WIDTH = 512
CONF_KERNEL = 31
NA_WIDTH = NA_HEADS * HEAD_DIM
WA_Q_WIDTH = WA_Q_HEADS * HEAD_DIM
WA_KV_WIDTH = WA_KV_HEADS * HEAD_DIM
SSM_WIDTH = SSM_HEADS * SSM_HEAD_DIM
SSM_BC_WIDTH = SSM_GROUPS * SSM_STATE
SSM_XBC_WIDTH = SSM_WIDTH + 2 * SSM_BC_WIDTH
D_MIX = NA_WIDTH + WA_Q_WIDTH + SSM_WIDTH + CONF_WIDTH
IN_SPLITS = (NA_WIDTH, NA_WIDTH, NA_WIDTH, WA_Q_WIDTH, WA_KV_WIDTH, WA_KV_WIDTH,
             SSM_WIDTH, SSM_XBC_WIDTH, 2 * SSM_HEADS, 2 * CONF_WIDTH)
IN_WIDTH = (3 * NA_WIDTH + WA_Q_WIDTH + 2 * WA_KV_WIDTH + SSM_WIDTH + SSM_XBC_WIDTH
            + 2 * SSM_HEADS + 2 * CONF_WIDTH)
N_EXPERTS = 16
EC_CAPACITY = 2
EXPERT_FF = D_MODEL // 2

kernel_name = 'hybrid_na2d_swa_ssd_conv_ec_encoder'


def _rms(x, w):
    xf = x.astype(jnp.float32)
    y = xf * lax.rsqrt(jnp.mean(xf * xf, axis=-1, keepdims=True) + EPS)
    return (y * w.astype(jnp.float32)).astype(x.dtype)


def _layer_norm(x, w, b):
    xf = x.astype(jnp.float32)
    mu = jnp.mean(xf, axis=-1, keepdims=True)
    var = jnp.mean(jnp.square(xf - mu), axis=-1, keepdims=True)
    y = (xf - mu) * lax.rsqrt(var + EPS)
    return (y * w.astype(jnp.float32) + b.astype(jnp.float32)).astype(x.dtype)


def _dwconv(x, w, b):
    k = w.shape[0]
    left = (k - 1) // 2
    y = lax.conv_general_dilated(x, w[:, None, :].astype(x.dtype), window_strides=(1,),
                                 padding=[(left, k - 1 - left)],
                                 dimension_numbers=('NWC', 'WIO', 'NWC'),
                                 feature_group_count=x.shape[-1])
    return y + b.astype(x.dtype)


def _split_cols(u):
    offs = np.cumsum(np.array(IN_SPLITS))[:-1].tolist()
    return jnp.split(u, offs, axis=-1)


def _partial_rope(x, pos):
    half = ROPE_DIM // 2
    inv_freq = 1.0 / (ROPE_THETA ** (jnp.arange(half, dtype=jnp.float32) * 2.0 / ROPE_DIM))
    ang = pos.astype(jnp.float32)[:, None] * inv_freq[None, :]
    cos = jnp.cos(ang)[None, :, None, :]
    sin = jnp.sin(ang)[None, :, None, :]
    xf = x[..., :ROPE_DIM].astype(jnp.float32)
    x1, x2 = xf[..., :half], xf[..., half:]
    rot = jnp.concatenate([x1 * cos - x2 * sin, x2 * cos + x1 * sin], axis=-1).astype(x.dtype)
    return jnp.concatenate([rot, x[..., ROPE_DIM:]], axis=-1)


def _neighbourhood_attention(q, k, v, rpb):
    bsz, s, h, d = q.shape
    rows = s // GRID_W
    kh = min(NA_KH, rows)
    q = (q * d ** -0.5).reshape(bsz, rows, GRID_W, h, d)
    k = k.reshape(bsz, rows, GRID_W, h, d)
    v = v.reshape(bsz, rows, GRID_W, h, d)
    col = jnp.arange(GRID_W)
    col_idx = jnp.clip(col - NA_KW // 2, 0, GRID_W - NA_KW)[:, None] + jnp.arange(NA_KW)[None, :]
    bias_cols = rpb[:, :, col_idx - col[:, None] + NA_KW - 1]

    def one_row(r):
        r0 = jnp.clip(r - kh // 2, 0, rows - kh)
        k_win = lax.dynamic_slice_in_dim(k, r0, kh, axis=1)[:, :, col_idx]
        v_win = lax.dynamic_slice_in_dim(v, r0, kh, axis=1)[:, :, col_idx]
        q_r = lax.dynamic_index_in_dim(q, r, axis=1, keepdims=False)
        bias = bias_cols[:, r0 + jnp.arange(kh) - r + NA_KH - 1]
        logits = (jnp.einsum('bqhd,brqkhd->bhqrk', q_r, k_win).astype(jnp.float32)
                  + jnp.transpose(bias, (0, 2, 1, 3)).astype(jnp.float32))
        p = jax.nn.softmax(logits.reshape(bsz, h, GRID_W, kh * NA_KW), axis=-1).reshape(logits.shape)
        return jnp.einsum('bhqrk,brqkhd->bqhd', p.astype(v.dtype), v_win)

    out = lax.map(one_row, jnp.arange(rows))
    return jnp.transpose(out, (1, 0, 2, 3, 4)).reshape(bsz, s, h * d)


def _window_attention(q, k, v, sink):
    bsz, s, hq, d = q.shape
    hkv = k.shape[2]
    g = hq // hkv
    nb = s // WA_BLOCK
    qb = (q * d ** -0.5).reshape(bsz, nb, WA_BLOCK, hkv, g, d)

    def band(t):
        tp = jnp.pad(t, ((0, 0), (WA_BLOCK, WA_BLOCK), (0, 0), (0, 0))).reshape(bsz, nb + 2, WA_BLOCK, hkv, d)
        return jnp.concatenate([tp[:, :-2], tp[:, 1:-1], tp[:, 2:]], axis=2)

    kb, vb = band(k), band(v)
    qi = jnp.arange(WA_BLOCK)[:, None]
    kj = jnp.arange(3 * WA_BLOCK)[None, :]
    kpos = (jnp.arange(nb) * WA_BLOCK - WA_BLOCK)[:, None, None] + kj[None]
    mask = (jnp.abs(kj - WA_BLOCK - qi)[None] <= WINDOW) & (kpos >= 0) & (kpos < s)
    logits = jnp.einsum('bnqhgd,bnkhd->bhgnqk', qb, kb).astype(jnp.float32)
    logits = jnp.where(mask, logits, -jnp.inf)
    sink_l = jnp.broadcast_to(sink.astype(jnp.float32).reshape(1, hkv, g, 1, 1, 1), logits.shape[:-1] + (1,))
    p = jax.nn.softmax(jnp.concatenate([logits, sink_l], axis=-1), axis=-1)[..., :-1]
    o = jnp.einsum('bhgnqk,bnkhd->bnqhgd', p.astype(v.dtype), vb)
    return o.reshape(bsz, s, hq * d)


def _ssd(x, dt, a, bm, cm):
    bsz, s, h, p = x.shape
    L = SSM_CHUNK
    nc = s // L
    hg = h // SSM_GROUPS
    xc = (x * dt[..., None]).reshape(bsz, nc, L, SSM_GROUPS, hg, p)
    ac = (dt * a).reshape(bsz, nc, L, SSM_GROUPS, hg)
    bc = bm.reshape(bsz, nc, L, SSM_GROUPS, SSM_STATE)
    cc = cm.reshape(bsz, nc, L, SSM_GROUPS, SSM_STATE)
    a_cum = jnp.cumsum(ac, axis=2)
    tri = jnp.tril(jnp.ones((L, L), dtype=bool))[None, None, :, :, None, None]
    seg = a_cum[:, :, :, None] - a_cum[:, :, None, :]
    decay = jnp.exp(jnp.where(tri, seg, -jnp.inf))
    cb = jnp.einsum('bclgn,bcsgn->bclsg', cc, bc)
    y_diag = jnp.einsum('bclsg,bclsgh,bcsghp->bclghp', cb, decay, xc)
    decay_states = jnp.exp(a_cum[:, :, -1:] - a_cum)
    states = jnp.einsum('bclgn,bclgh,bclghp->bcghpn', bc, decay_states, xc)
    chunk_decay = jnp.exp(a_cum[:, :, -1])

    def step(hstate, inp):
        dec, st = inp
        return dec[..., None, None] * hstate + st, hstate

    h0 = jnp.zeros((bsz, SSM_GROUPS, hg, p, SSM_STATE), jnp.float32)
    _, prev = lax.scan(step, h0, (jnp.moveaxis(chunk_decay, 1, 0), jnp.moveaxis(states, 1, 0)))
    prev = jnp.moveaxis(prev, 0, 1)
    y_off = jnp.einsum('bclgn,bcghpn,bclgh->bclghp', cc, prev, jnp.exp(a_cum))
    return (y_diag + y_off).reshape(bsz, s, h, p)


def _expert_choice_ffn(h, w_router, w_gate, w_up, w_down):
    bsz, s, dm = h.shape
    cap = EC_CAPACITY * s // N_EXPERTS
    aff = jax.nn.softmax(jnp.einsum('bsd,de->bse', h, w_router).astype(jnp.float32), axis=-1)
    gate, idx = lax.top_k(jnp.swapaxes(aff, 1, 2), cap)
    xin = jax.vmap(lambda hb, ib: hb[ib])(h, idx)
    hid = jax.nn.silu(jnp.einsum('becd,edf->becf', xin, w_gate)) * jnp.einsum('becd,edf->becf', xin, w_up)
    out = jnp.einsum('becf,efd->becd', hid, w_down) * gate[..., None].astype(h.dtype)
    return jax.vmap(lambda ob, ib: jnp.zeros((s, dm), ob.dtype).at[ib.reshape(-1)].add(ob.reshape(-1, dm)))(out, idx)


def _mixing_sublayer(x, norm_w, w_in, na_q_norm, na_k_norm, na_rpb, wa_q_norm, wa_k_norm, wa_sink,
                     ssm_conv_w, ssm_conv_b, ssm_dt_bias, ssm_a_log, ssm_d, ssm_norm_w,
                     conf_dw_w, conf_dw_b, conf_ln_w, conf_ln_b, w_out):
    bsz, s, _ = x.shape
    hn = _rms(x, norm_w)
    u = jnp.einsum('bsd,de->bse', hn, w_in)
    na_q, na_k, na_v, wa_q, wa_k, wa_v, ssm_z, ssm_xbc, ssm_dt, conf_in = _split_cols(u)

    def heads(t):
        return t.reshape(bsz, s, -1, HEAD_DIM)

    o_a = _neighbourhood_attention(_rms(heads(na_q), na_q_norm), _rms(heads(na_k), na_k_norm), heads(na_v), na_rpb)

    pos = jnp.arange(s)
    o_b = _window_attention(_partial_rope(_rms(heads(wa_q), wa_q_norm), pos),
                            _partial_rope(_rms(heads(wa_k), wa_k_norm), pos),
                            heads(wa_v), wa_sink)

    xbc = jax.nn.silu(_dwconv(ssm_xbc, ssm_conv_w, ssm_conv_b)).astype(jnp.float32)
    xs = xbc[..., :SSM_WIDTH].reshape(bsz, s, SSM_HEADS, SSM_HEAD_DIM)
    bm = xbc[..., SSM_WIDTH:SSM_WIDTH + SSM_BC_WIDTH].reshape(bsz, s, SSM_GROUPS, SSM_STATE)
    cm = xbc[..., SSM_WIDTH + SSM_BC_WIDTH:].reshape(bsz, s, SSM_GROUPS, SSM_STATE)
    dt = jax.nn.softplus(ssm_dt.astype(jnp.float32).reshape(bsz, s, 2, SSM_HEADS) + ssm_dt_bias.astype(jnp.float32))
    a = -jnp.exp(ssm_a_log.astype(jnp.float32))

    def flip(t):
        return jnp.flip(t, axis=1)

    y_fwd = _ssd(xs, dt[:, :, 0], a[0], bm, cm)
    y_bwd = flip(_ssd(flip(xs), flip(dt[:, :, 1]), a[1], flip(bm), flip(cm)))
    y = (y_fwd + y_bwd + xs * ssm_d.astype(jnp.float32)[:, None]).reshape(bsz, s, SSM_WIDTH).astype(x.dtype)
    o_c = _rms(y * jax.nn.silu(ssm_z), ssm_norm_w)

    c_a, c_g = jnp.split(conf_in, 2, axis=-1)
    cv = _dwconv(c_a * jax.nn.sigmoid(c_g), conf_dw_w, conf_dw_b)
    o_d = jax.nn.silu(_layer_norm(cv, conf_ln_w, conf_ln_b))

    mixed = jnp.concatenate([o_a, o_b, o_c, o_d], axis=-1)
    return x + jnp.einsum('bse,ed->bsd', mixed, w_out)


def setup_inputs(seed: int = 0) -> dict:
    key = jax.random.key(seed)
    ks = jax.random.split(key, 26)
    f32 = jnp.float32
    L = DEPTH

    def nrm(k, shape, sc):
        return jax.random.normal(k, shape, f32) * sc

    def gain(k, shape):
        return 1.0 + 0.02 * jax.random.normal(k, shape, f32)

    dt0 = jnp.exp(jax.random.uniform(ks[12], (L, 2, SSM_HEADS), f32, math.log(1e-3), math.log(1e-1)))
    return {
        'x': jax.random.normal(ks[0], (BATCH, SEQ, D_MODEL), f32),
        'mix_norm_w': gain(ks[1], (L, D_MODEL)),
        'w_in': nrm(ks[2], (L, D_MODEL, IN_WIDTH), D_MODEL ** -0.5),
        'na_q_norm': gain(ks[3], (L, HEAD_DIM)),
        'na_k_norm': gain(ks[4], (L, HEAD_DIM)),
        'na_rpb': nrm(ks[5], (L, NA_HEADS, 2 * NA_KH - 1, 2 * NA_KW - 1), 0.1),
        'wa_q_norm': gain(ks[6], (L, HEAD_DIM)),
        'wa_k_norm': gain(ks[7], (L, HEAD_DIM)),
        'wa_sink': nrm(ks[8], (L, WA_Q_HEADS), 0.5),
        'ssm_conv_w': nrm(ks[9], (L, SSM_CONV, SSM_XBC_WIDTH), SSM_CONV ** -0.5),
        'ssm_conv_b': nrm(ks[10], (L, SSM_XBC_WIDTH), 0.02),
        'ssm_dt_bias': dt0 + jnp.log(-jnp.expm1(-dt0)),
        'ssm_a_log': jnp.log(jax.random.uniform(ks[13], (L, 2, SSM_HEADS), f32, 1.0, 16.0)),
        'ssm_d': gain(ks[14], (L, SSM_HEADS)),
        'ssm_norm_w': gain(ks[15], (L, SSM_WIDTH)),
        'conf_dw_w': nrm(ks[16], (L, CONF_KERNEL, CONF_WIDTH), CONF_KERNEL ** -0.5),
        'conf_dw_b': nrm(ks[17], (L, CONF_WIDTH), 0.02),
        'conf_ln_w': gain(ks[18], (L, CONF_WIDTH)),
        'conf_ln_b': nrm(ks[19], (L, CONF_WIDTH), 0.02),
        'w_out': nrm(ks[20], (L, D_MIX, D_MODEL), D_MIX ** -0.5),
        'ffn_norm_w': gain(ks[21], (L, D_MODEL)),
        'w_router': nrm(ks[22], (L, D_MODEL, N_EXPERTS), D_MODEL ** -0.5),
        'w_gate': nrm(ks[23], (L, N_EXPERTS, D_MODEL, EXPERT_FF), D_MODEL ** -0.5),
        'w_up': nrm(ks[24], (L, N_EXPERTS, D_MODEL, EXPERT_FF), D_MODEL ** -0.5),
        'w_down': nrm(ks[25], (L, N_EXPERTS, EXPERT_FF, D_MODEL), EXPERT_FF ** -0.5),
    }


def reference(x, mix_norm_w, w_in, na_q_norm, na_k_norm, na_rpb, wa_q_norm, wa_k_norm, wa_sink,
              ssm_conv_w, ssm_conv_b, ssm_dt_bias, ssm_a_log, ssm_d, ssm_norm_w,
              conf_dw_w, conf_dw_b, conf_ln_w, conf_ln_b, w_out,
              ffn_norm_w, w_router, w_gate, w_up, w_down):
    for l in range(DEPTH):
        x = _mixing_sublayer(x, mix_norm_w[l], w_in[l], na_q_norm[l], na_k_norm[l], na_rpb[l],
                             wa_q_norm[l], wa_k_norm[l], wa_sink[l],
                             ssm_conv_w[l], ssm_conv_b[l], ssm_dt_bias[l], ssm_a_log[l], ssm_d[l], ssm_norm_w[l],
                             conf_dw_w[l], conf_dw_b[l], conf_ln_w[l], conf_ln_b[l], w_out[l])
        x = x + _expert_choice_ffn(_rms(x, ffn_norm_w[l]), w_router[l], w_gate[l], w_up[l], w_down[l])
    return x
```

```python
import numpy as np
import ml_dtypes
from contextlib import ExitStack
import concourse.bass as bass
import concourse.mybir as mybir
from concourse.bass_utils import run_bass_kernel_spmd

F32 = mybir.dt.float32
BF16 = mybir.dt.bfloat16
I32 = mybir.dt.int32
AF = mybir.ActivationFunctionType
ALU = mybir.AluOpType
AX = mybir.AxisListType
NPBF = ml_dtypes.bfloat16

ENG = ("sync", "gpsimd", "tensor", "vector", "scalar")


class Buf:
    __slots__ = ("name", "w", "r")

    def __init__(self, name):
        self.name = name
        self.w = None
        self.r = []


class Prog:
    def __init__(self):
        self.nc = bass.Bass("TRN2", target_bir_lowering=False)
        self.es = ExitStack()
        self.q = {e: [] for e in ENG}
        self.cnt = {}
        self.semh = {}
        self.pe_pending = []

    def dram(self, name, shape, dt, kind="Internal"):
        return self.nc.dram_tensor(name, list(shape), dt, kind=kind).ap()

    def sb(self, name, shape, dt):
        return self.es.enter_context(self.nc.sbuf_tensor(name, list(shape), dt))

    def ps(self, name, shape, dt=F32):
        return self.es.enter_context(self.nc.psum_tensor(name, list(shape), dt))

    def _sem(self, key):
        if key not in self.semh:
            nm = "s" + str(len(self.semh))
            self.semh[key] = self.es.enter_context(self.nc.semaphore(nm))
            self.cnt[key] = 0
        return self.semh[key]

    def _deps(self, eng, reads, writes):
        waits = []
        for b in reads:
            if b.w is not None:
                waits.append(b.w)
        for b in writes:
            if b.w is not None:
                waits.append(b.w)
            for t in b.r:
                waits.append(t)
        return waits

    def op(self, eng, fn, reads=(), writes=(), count=True):
        waits = self._deps(eng, reads, writes)
        if eng == "tensor":
            waits = [w for w in waits if w[0] != "tensor"]
        if count:
            self._sem(eng)
            self.cnt[eng] += 1
            tok = (eng, self.cnt[eng])
        else:
            tok = None
        self.q[eng].append((fn, waits, eng if count else None, 1))
        if eng == "tensor":
            if tok is None:
                self.pe_pending.extend(reads)
                return None
            for b in self.pe_pending:
                b.r.append(tok)
            self.pe_pending = []
        for b in reads:
            b.r.append(tok)
        for b in writes:
            b.w = tok
            b.r = []
        return tok

    def dma(self, eng, out, in_, key, reads=(), writes=(), **kw):
        waits = self._deps(eng, reads, writes)
        k = ("d", key)
        self._sem(k)
        self.cnt[k] += 16
        tok = (k, self.cnt[k])
        self.q[eng].append((lambda e: e.dma_start(out=out, in_=in_, **kw), waits, k, 16))
        for b in reads:
            b.r.append(tok)
        for b in writes:
            b.w = tok
            b.r = []
        return tok

    def raw(self, eng, fn, key, inc, reads=(), writes=()):
        waits = self._deps(eng, reads, writes)
        k = ("d", key)
        self._sem(k)
        self.cnt[k] += inc
        tok = (k, self.cnt[k])
        self.q[eng].append((fn, waits, k, inc))
        for b in reads:
            b.r.append(tok)
        for b in writes:
            b.w = tok
            b.r = []
        return tok

    def finish(self):
        finals = [(k, v) for k, v in self.cnt.items() if isinstance(k, tuple)]
        semh = self.semh
        with self.nc.Block() as block:
            for eng in ENG:
                items = self.q[eng]
                if not items and eng != "sync":
                    continue

                def body(e, items=items, eng=eng):
                    waited = {}
                    for fn, waits, key, inc in items:
                        for (k, v) in waits:
                            if waited.get(k, 0) >= v:
                                continue
                            e.wait_ge(semh[k], v)
                            waited[k] = v
                        ins = fn(e)
                        if key is not None:
                            ins.then_inc(semh[key], inc)
                    if eng == "sync":
                        for k, v in finals:
                            e.wait_ge(semh[k], v)
                getattr(block, eng)(body)
        self.es.close()
        return self.nc


def run(nc, in_maps, trace=False):
    res = run_bass_kernel_spmd(nc, in_maps, core_ids=list(range(len(in_maps))), trace=trace)
    return res


D = 2048
INW = 4880
KC = D // 128
EPS = 1e-6
FM_RANGES = [(0, 1024), (1536, 640), (2304, 1536), (3856, 1024)]
NFM = sum(w for _, w in FM_RANGES) // 128
TM_RANGES = [(1024, 512), (2176, 128)]
DT_RANGE = (3840, 16)


def build_k1(T=2048):
    p = Prog()
    nc = p.nc
    xT = p.dram("xT", [D, T], F32, "ExternalInput")
    nw = p.dram("nw", [128, KC], F32, "ExternalInput")
    w_in = p.dram("w_in", [D, INW], F32, "ExternalInput")
    ufm = p.dram("ufm", [NFM * 128, T], BF16, "ExternalOutput")
    utm = p.dram("utm", [T, 640], BF16, "ExternalOutput")
    udt = p.dram("udt", [T, 16], F32, "ExternalOutput")

    TT = 512
    NT = T // TT
    hnT = p.sb("hnT", [128, KC, T], BF16)
    b_hn = [Buf("hn%d" % i) for i in range(NT)]
    nwt = p.sb("nwt", [128, KC], F32)
    b_nw = Buf("nw")
    ones = p.sb("ones", [128, 128], BF16)
    b_ones = Buf("ones")
    xt = [p.sb("xt%d" % i, [128, KC, TT], F32) for i in range(2)]
    b_xt = [Buf("xt%d" % i) for i in range(2)]
    sq = p.sb("sq", [128, KC, TT], BF16)
    b_sq = Buf("sq")
    rstd = p.sb("rstd", [128, TT], F32)
    b_rstd = Buf("rstd")
    ps_ss = p.ps("ps_ss", [128, TT])
    b_pss = Buf("pss")
    pss = [p.ps("psm%d" % i, [128, 512]) for i in range(4)]
    b_ps = [Buf("psm%d" % i) for i in range(4)]
    wp = [p.sb("wp%d" % i, [128, KC, 512], BF16) for i in range(2)]
    b_wp = [Buf("wp%d" % i) for i in range(2)]
    ost = [p.sb("ost%d" % i, [128, T], BF16) for i in range(2)]
    b_ost = [Buf("ost%d" % i) for i in range(2)]
    otm = [p.sb("otm%d" % i, [128, 512], BF16) for i in range(2)]
    b_otm = [Buf("otm%d" % i) for i in range(2)]
    odt = [p.sb("odt%d" % i, [128, 16], F32) for i in range(2)]
    b_odt = [Buf("odt%d" % i) for i in range(2)]

    epst = p.sb("epst", [128, 1], F32)
    b_eps = Buf("eps")
    p.op("gpsimd", lambda e: e.memset(epst[:], EPS), writes=[b_eps])
    p.dma("sync", nwt[:], nw, "nw", writes=[b_nw])
    p.op("gpsimd", lambda e: e.memset(ones[:], 1.0), writes=[b_ones])

    xT_v = xT.rearrange("(k p) t -> p k t", p=128)
    w_v = w_in.rearrange("(k p) n -> p k n", p=128)

    for tt in range(NT):
        s = tt % 2
        p.dma("sync", xt[s][:], xT_v[:, :, tt * TT:(tt + 1) * TT], "xt%d" % s, writes=[b_xt[s]])
        p.op("scalar", lambda e, s=s: e.activation(out=sq[:], in_=xt[s][:], func=AF.Square),
             reads=[b_xt[s]], writes=[b_sq])
        for k in range(KC):
            p.op("tensor", lambda e, k=k: e.matmul(ps_ss[:], lhsT=ones[:], rhs=sq[:, k, :],
                                                    start=(k == 0), stop=(k == KC - 1)),
                 reads=[b_ones, b_sq], writes=[b_pss], count=(k == KC - 1))
        p.op("scalar", lambda e: e.activation(out=rstd[:], in_=ps_ss[:], func=AF.Sqrt, bias=epst[:], scale=1.0 / D),
             reads=[b_pss, b_eps], writes=[b_rstd])
        p.op("vector", lambda e: e.reciprocal(out=rstd[:], in_=rstd[:]),
             reads=[b_rstd], writes=[b_rstd])
        for k in range(KC):
            eng = "vector"
            p.op(eng, lambda e, k=k, s=s, tt=tt: e.scalar_tensor_tensor(
                out=hnT[:, k, tt * TT:(tt + 1) * TT], in0=xt[s][:, k, :], scalar=nwt[:, k:k + 1],
                in1=rstd[:], op0=ALU.mult, op1=ALU.mult),
                reads=[b_xt[s], b_rstd, b_nw], writes=[b_hn[tt]])

    panels = []
    row = 0
    for c0, w in FM_RANGES:
        o = 0
        while o < w:
            pw = min(512, w - o)
            panels.append((c0 + o, pw, "fm", row))
            row += pw // 128
            o += pw
    panels.append((TM_RANGES[0][0], 512, "tm", 0))
    panels.append((TM_RANGES[1][0], 128, "tm", 512))
    panels.append((DT_RANGE[0], 16, "dt", 0))

    ev = 0
    psi = 0
    osi = 0
    for pi, (c0, pw, kind, o0) in enumerate(panels):
        s = pi % 2
        p.dma("gpsimd", wp[s][:, :, 0:pw], w_v[:, :, c0:c0 + pw], "wp%d" % s, writes=[b_wp[s]])
        if kind == "fm":
            for ci in range(pw // 128):
                so = osi % 2
                osi += 1
                for tt in range(NT):
                    b = psi % 4
                    psi += 1
                    for k in range(KC):
                        p.op("tensor", lambda e, b=b, s=s, ci=ci, k=k, tt=tt: e.matmul(
                            pss[b][:], lhsT=wp[s][:, k, ci * 128:(ci + 1) * 128],
                            rhs=hnT[:, k, tt * TT:(tt + 1) * TT], start=(k == 0), stop=(k == KC - 1)),
                            reads=[b_wp[s], b_hn[tt]], writes=[b_ps[b]], count=(k == KC - 1))
                    eng = "scalar" if ev % 2 == 0 else "vector"
                    ev += 1
                    if eng == "scalar":
                        p.op(eng, lambda e, b=b, so=so, tt=tt: e.copy(out=ost[so][:, tt * TT:(tt + 1) * TT], in_=pss[b][:]),
                             reads=[b_ps[b]], writes=[b_ost[so]])
                    else:
                        p.op(eng, lambda e, b=b, so=so, tt=tt: e.tensor_copy(out=ost[so][:, tt * TT:(tt + 1) * TT], in_=pss[b][:]),
                             reads=[b_ps[b]], writes=[b_ost[so]])
                r0 = (o0 + ci) * 128
                p.dma("sync", ufm[r0:r0 + 128, :], ost[so][:], "ost%d" % so, reads=[b_ost[so]])
        else:
            for t8 in range(T // 128):
                b = psi % 4
                psi += 1
                for k in range(KC):
                    p.op("tensor", lambda e, b=b, s=s, k=k, t8=t8, pw=pw: e.matmul(
                        pss[b][:, 0:pw], lhsT=hnT[:, k, t8 * 128:(t8 + 1) * 128],
                        rhs=wp[s][:, k, 0:pw], start=(k == 0), stop=(k == KC - 1)),
                        reads=[b_wp[s], b_hn[t8 // 4]], writes=[b_ps[b]], count=(k == KC - 1))
                so = t8 % 2
                if kind == "tm":
                    p.op("vector", lambda e, b=b, so=so, pw=pw: e.tensor_copy(out=otm[so][:, 0:pw], in_=pss[b][:, 0:pw]),
                         reads=[b_ps[b]], writes=[b_otm[so]])
                    p.dma("sync", utm[t8 * 128:(t8 + 1) * 128, o0:o0 + pw], otm[so][:, 0:pw], "otm%d" % so,
                          reads=[b_otm[so]])
                else:
                    p.op("vector", lambda e, b=b, so=so, pw=pw: e.tensor_copy(out=odt[so][:, 0:pw], in_=pss[b][:, 0:pw]),
                         reads=[b_ps[b]], writes=[b_odt[so]])
                    p.dma("sync", udt[t8 * 128:(t8 + 1) * 128, :], odt[so][:], "odt%d" % so, reads=[b_odt[so]])
    return p.finish()


S = 8192
NQT = S // 128
EPS = 1e-6
NEG = -30000.0


def na_tiles():
    out = []
    for qp in range(NQT):
        if qp == 0:
            out.append(([0, 1, 2, 3], [5, 6, 7, 8]))
        elif qp == 1:
            out.append(([0, 1, 2, 3], [9, 10, 11, 12]))
        elif qp == 62:
            out.append(([60, 61, 62, 63], [13, 14, 15, 16]))
        elif qp == 63:
            out.append(([60, 61, 62, 63], [17, 18, 19, 20]))
        else:
            out.append(([qp - 2, qp - 1, qp, qp + 1, qp + 2], [0, 1, 2, 3, 4]))
    return out


def na_bias_host(rpb):
    H = rpb.shape[0]
    tl = na_tiles()
    cases = [(2, 0)] * 0
    res = np.full((H, 21, 128, 128), NEG, np.float32)
    done = set()
    for qp, (kps, tis) in enumerate(tl):
        for kp, ti in zip(kps, tis):
            if ti in done:
                continue
            done.add(ti)
            qi = np.arange(128)
            r = 2 * qp + qi // 64
            c = qi % 64
            r0 = np.clip(r - 4, 0, 120)
            c0 = np.clip(c - 8, 0, 48)
            ki = np.arange(128)
            kr = 2 * kp + ki // 64
            kc = ki % 64
            inwin = ((kr[:, None] >= r0[None, :]) & (kr[:, None] < r0[None, :] + 8) &
                     (kc[:, None] >= c0[None, :]) & (kc[:, None] < c0[None, :] + 16))
            di = np.clip(kr[:, None] - r[None, :] + 7, 0, 14)
            dj = np.clip(kc[:, None] - c[None, :] + 15, 0, 30)
            vals = rpb[:, di, dj]
            res[:, ti] = np.where(inwin[None], vals, np.float32(NEG))
    return res


def wa_mask_host():
    ki = np.arange(128)[:, None]
    qi = np.arange(128)[None, :]
    m = np.zeros((3, 128, 128), np.float32)
    m[0] = np.where(ki >= qi, 0.0, NEG)
    m[2] = np.where(ki <= qi, 0.0, NEG)
    return m


def rope_tables_host():
    half = 8
    inv = 1.0 / (500000.0 ** (np.arange(half, dtype=np.float32) * 2.0 / 16))
    ang = np.arange(S, dtype=np.float32)[None, :] * inv[:, None].astype(np.float32)
    cos = np.cos(ang).astype(np.float32)
    sin = np.sin(ang).astype(np.float32)
    cT = np.ones((64, S), np.float32)
    sT = np.zeros((64, S), np.float32)
    cT[0:8] = cos
    cT[8:16] = cos
    sT[0:8] = -sin
    sT[8:16] = sin
    pm = np.zeros((64, 64), np.float32)
    for i in range(8):
        pm[i, i + 8] = 1.0
        pm[i + 8, i] = 1.0
    return cT, sT, pm


def build_k2a():
    p = Prog()
    na_qT = p.dram("na_qT", [128, S], BF16, "ExternalInput")
    na_kT = p.dram("na_kT", [128, S], BF16, "ExternalInput")
    na_v = p.dram("na_v", [S, 128], BF16, "ExternalInput")
    wa_qT = p.dram("wa_qT", [128, S], BF16, "ExternalInput")
    wa_kT = p.dram("wa_kT", [64, S], BF16, "ExternalInput")
    wa_v = p.dram("wa_v", [S, 64], BF16, "ExternalInput")
    nrm = p.dram("nrm", [128, 4], F32, "ExternalInput")
    na_bias = p.dram("na_bias", [128, 2 * 21 * 128], F32, "ExternalInput")
    wa_mask = p.dram("wa_mask", [128, 3 * 128], F32, "ExternalInput")
    cst = p.dram("cst", [128, 3 * 128], F32, "ExternalInput")
    ropeT = p.dram("ropeT", [128, 2 * S], F32, "ExternalInput")
    sink = p.dram("sink", [128, 2], F32, "ExternalInput")
    o_na = p.dram("o_na", [S, 128], BF16, "ExternalOutput")
    o_wa = p.dram("o_wa", [S, 128], BF16, "ExternalOutput")

    qn = p.sb("qn", [128, S], BF16); b_qn = Buf("qn")
    kn = p.sb("kn", [128, S], BF16); b_kn = Buf("kn")
    raw = [p.sb("raw%d" % i, [128, S], BF16) for i in range(2)]; b_raw = [Buf("raw0"), Buf("raw1")]
    vaug = p.sb("vaug", [128, NQT, 2, 65], BF16); b_va = Buf("vaug")
    vraw = p.sb("vraw", [128, NQT, 128], BF16); b_vr = Buf("vraw")
    nrt = p.sb("nrt", [128, 4], F32); b_nr = Buf("nrt")
    nr8 = p.sb("nr8", [128, 4], F32); b_nr8 = Buf("nr8")
    bias = p.sb("bias", [128, 2 * 21 * 128], BF16); b_bias = Buf("bias")
    wmask = p.sb("wmask", [128, 3 * 128], BF16); b_wm = Buf("wmask")
    cs = p.sb("cs", [128, 3 * 128], BF16); b_cs = Buf("cs")
    ident = cs[:, 0:128]
    bones = cs[:, 128:256]
    pmat = cs[:, 256:384]
    skt = p.sb("skt", [128, 2], F32); b_sk = Buf("skt")
    epst = p.sb("epst", [128, 1], F32); b_eps = Buf("eps")
    sq = [p.sb("sq%d" % i, [128, 512], BF16) for i in range(2)]; b_sq = [Buf("sq0"), Buf("sq1")]
    rstd = [p.sb("rstd%d" % i, [128, 512], F32) for i in range(2)]; b_rstd = [Buf("r0"), Buf("r1")]
    tmpn = [p.sb("tmpn%d" % i, [128, 512], F32) for i in range(2)]; b_tmpn = [Buf("t0"), Buf("t1")]
    rp = [p.sb("rp%d" % i, [128, 2, 512], F32) for i in range(2)]; b_rp = [Buf("rp0"), Buf("rp1")]
    ps_n = [p.ps("ps_n%d" % i, [128, 512]) for i in range(2)]; b_psn = [Buf("psn0"), Buf("psn1")]
    ps_s = [p.ps("ps_s%d" % i, [128, 1024]) for i in range(2)]; b_pss = [Buf("pss0"), Buf("pss1")]
    ps_o = [p.ps("ps_o%d" % i, [128, 128]) for i in range(2)]; b_pso = [Buf("pso0"), Buf("pso1")]
    E = [p.sb("E%d" % i, [128, 640], BF16) for i in range(2)]; b_E = [Buf("E0"), Buf("E1")]
    den = [p.sb("den%d" % i, [128, 1], F32) for i in range(2)]; b_den = [Buf("den0"), Buf("den1")]
    ost = [p.sb("ost%d" % i, [128, 8, 128], BF16) for i in range(2)]; b_ost = [Buf("ost0"), Buf("ost1")]

    p.op("gpsimd", lambda e: e.memset(epst[:], EPS), writes=[b_eps])
    p.dma("sync", nrt[:], nrm, "nrt", writes=[b_nr])
    p.dma("sync", skt[:], sink, "skt", writes=[b_sk])
    p.dma("gpsimd", cs[:], cst, "cs", writes=[b_cs])
    p.dma("gpsimd", bias[:], na_bias, "bias", writes=[b_bias])
    p.dma("gpsimd", wmask[:], wa_mask, "wm", writes=[b_wm])
    p.op("scalar", lambda e: e.activation(out=skt[:], in_=skt[:], func=AF.Exp), reads=[b_sk], writes=[b_sk])
    p.op("vector", lambda e: e.tensor_scalar(out=nr8[:], in0=nrt[:], scalar1=0.125, scalar2=None, op0=ALU.mult),
         reads=[b_nr], writes=[b_nr8])

    cnt = {"n": 0}

    def qknorm(src, nparts, dst, b_dst, wcol, rope):
        for tt in range(S // 512):
            i = cnt["n"] % 2
            cnt["n"] += 1
            sl = slice(tt * 512, (tt + 1) * 512)
            p.op("scalar", lambda e, i=i, sl=sl: e.activation(out=sq[i][0:nparts, :], in_=src[0:nparts, sl], func=AF.Square),
                 reads=[b_src], writes=[b_sq[i]])
            p.op("tensor", lambda e, i=i: e.matmul(ps_n[i][0:nparts, :], lhsT=bones[0:nparts, 0:nparts], rhs=sq[i][0:nparts, :],
                                                   start=True, stop=True),
                 reads=[b_cs, b_sq[i]], writes=[b_psn[i]])
            p.op("scalar", lambda e, i=i: e.activation(out=rstd[i][0:nparts, :], in_=ps_n[i][0:nparts, :], func=AF.Sqrt,
                                                       bias=epst[0:nparts, :], scale=1.0 / 64),
                 reads=[b_psn[i], b_eps], writes=[b_rstd[i]])
            p.op("vector", lambda e, i=i: e.reciprocal(out=rstd[i][0:nparts, :], in_=rstd[i][0:nparts, :]),
                 reads=[b_rstd[i]], writes=[b_rstd[i]])
            if not rope:
                p.op("vector", lambda e, i=i, sl=sl: e.scalar_tensor_tensor(
                    out=dst[0:nparts, sl], in0=src[0:nparts, sl], scalar=wcol[0:nparts, :], in1=rstd[i][0:nparts, :],
                    op0=ALU.mult, op1=ALU.mult), reads=[b_src, b_rstd[i], b_nr, b_nr8], writes=[b_dst])
            else:
                p.dma("sync", rp[i][0:nparts, 0, :], ropeT[0:nparts, sl], "rp%d" % i, writes=[b_rp[i]])
                p.dma("sync", rp[i][0:nparts, 1, :], ropeT[0:nparts, S + tt * 512:S + (tt + 1) * 512], "rp%d" % i,
                      writes=[b_rp[i]])
                p.op("vector", lambda e, i=i, sl=sl: e.scalar_tensor_tensor(
                    out=sq[i][0:nparts, :], in0=src[0:nparts, sl], scalar=wcol[0:nparts, :], in1=rstd[i][0:nparts, :],
                    op0=ALU.mult, op1=ALU.mult), reads=[b_src, b_rstd[i], b_nr, b_nr8, b_psn[i]], writes=[b_sq[i]])
                p.op("tensor", lambda e, i=i: e.matmul(ps_n[i][0:nparts, :], lhsT=pmat[0:nparts, 0:nparts], rhs=sq[i][0:nparts, :],
                                                       start=True, stop=True),
                     reads=[b_cs, b_sq[i]], writes=[b_psn[i]])
                p.op("vector", lambda e, i=i: e.tensor_tensor(out=tmpn[i][0:nparts, :], in0=ps_n[i][0:nparts, :],
                                                              in1=rp[i][0:nparts, 1, :], op=ALU.mult),
                     reads=[b_psn[i], b_rp[i]], writes=[b_tmpn[i]])
                p.op("gpsimd", lambda e, i=i: e.tensor_tensor(out=rstd[i][0:nparts, :], in0=sq[i][0:nparts, :],
                                                              in1=rp[i][0:nparts, 0, :], op=ALU.mult),
                     reads=[b_sq[i], b_rp[i]], writes=[b_rstd[i]])
                p.op("vector", lambda e, i=i, sl=sl: e.tensor_tensor(out=dst[0:nparts, sl], in0=rstd[i][0:nparts, :],
                                                                     in1=tmpn[i][0:nparts, :], op=ALU.add),
                     reads=[b_rstd[i], b_tmpn[i]], writes=[b_dst])

    def load_v(vsrc, width, nh):
        p.dma("sync", vraw[:, :, 0:width], vsrc.rearrange("(t p) c -> p t c", p=128), "vraw", writes=[b_vr])
        p.op("gpsimd", lambda e: e.memset(vaug[:, :, :, 64:65], 1.0), writes=[b_va])
        for h in range(2):
            c0 = h * 64 if nh == 2 else 0
            p.op("vector", lambda e, h=h, c0=c0: e.tensor_copy(out=vaug[:, :, h, 0:64], in_=vraw[:, :, c0:c0 + 64]),
                 reads=[b_vr], writes=[b_va])

    oc = {"n": 0, "e": 0}

    def attention(tiles_fn, kbase_fn, bias_fn, extra_den, o_dram):
        for qg in range(NQT // 8):
            so = oc["n"] % 2
            oc["n"] += 1
            for q8 in range(8):
                qp = qg * 8 + q8
                for h in range(2):
                    kps, bts = tiles_fn(qp)
                    nk = len(kps)
                    i = oc["e"] % 2
                    oc["e"] += 1
                    kb = kbase_fn(h)
                    for j, kp in enumerate(kps):
                        bt = bts[j]
                        last = bt is None
                        p.op("tensor", lambda e, i=i, j=j, kp=kp, kb=kb, h=h, qp=qp, last=last: e.matmul(
                            ps_s[i][:, j * 128:(j + 1) * 128], lhsT=kn[kb:kb + 64, kp * 128:(kp + 1) * 128],
                            rhs=qn[h * 64:(h + 1) * 64, qp * 128:(qp + 1) * 128], start=True, stop=last),
                            reads=[b_kn, b_qn], writes=[b_pss[i]], count=(last and j == nk - 1))
                        if not last:
                            p.op("tensor", lambda e, i=i, j=j, h=h, bt=bt: e.matmul(
                                ps_s[i][:, j * 128:(j + 1) * 128], lhsT=ident, rhs=bias_fn(h, bt), start=False, stop=True),
                                reads=[b_cs, b_bias, b_wm], writes=[b_pss[i]], count=(j == nk - 1))
                    p.op("scalar", lambda e, i=i, nk=nk: e.activation(out=E[i][:, 0:nk * 128], in_=ps_s[i][:, 0:nk * 128],
                                                                      func=AF.Exp),
                         reads=[b_pss[i]], writes=[b_E[i]])
                    for j, kp in enumerate(kps):
                        p.op("tensor", lambda e, i=i, j=j, kp=kp, h=h, nk=nk: e.matmul(
                            ps_o[i][:, 0:65], lhsT=E[i][:, j * 128:(j + 1) * 128], rhs=vaug[:, kp, h, :],
                            start=(j == 0), stop=(j == nk - 1)),
                            reads=[b_E[i], b_va], writes=[b_pso[i]], count=(j == nk - 1))
                    if extra_den:
                        p.op("vector", lambda e, i=i, h=h: e.tensor_scalar(out=den[i][:], in0=ps_o[i][:, 64:65],
                                                                           scalar1=skt[:, h:h + 1], scalar2=None, op0=ALU.add),
                             reads=[b_pso[i], b_sk], writes=[b_den[i]])
                        p.op("vector", lambda e, i=i: e.reciprocal(out=den[i][:], in_=den[i][:]),
                             reads=[b_den[i]], writes=[b_den[i]])
                    else:
                        p.op("vector", lambda e, i=i: e.reciprocal(out=den[i][:], in_=ps_o[i][:, 64:65]),
                             reads=[b_pso[i]], writes=[b_den[i]])
                    p.op("vector", lambda e, i=i, h=h, so=so, q8=q8: e.tensor_scalar(
                        out=ost[so][:, q8, h * 64:(h + 1) * 64], in0=ps_o[i][:, 0:64], scalar1=den[i][:], scalar2=None,
                        op0=ALU.mult), reads=[b_pso[i], b_den[i]], writes=[b_ost[so]])
            p.dma("sync", o_dram[qg * 1024:(qg + 1) * 1024, :].rearrange("(t p) c -> p t c", p=128), ost[so][:],
                  "ost%d" % so, reads=[b_ost[so]])

    b_src = Buf("src")
    p.dma("sync", raw[0][:], na_qT, "raw0", writes=[b_raw[0]])
    p.dma("sync", raw[1][:], na_kT, "raw1", writes=[b_raw[1]])
    load_v(na_v, 128, 2)
    b_src = b_raw[0]
    qknorm(raw[0], 128, qn, b_qn, nr8[:, 0:1], False)
    b_src = b_raw[1]
    qknorm(raw[1], 128, kn, b_kn, nrt[:, 1:2], False)
    tl = na_tiles()
    attention(lambda qp: tl[qp], lambda h: h * 64,
              lambda h, bt: bias[:, (h * 21 + bt) * 128:(h * 21 + bt + 1) * 128], False, o_na)

    p.dma("sync", raw[0][:], wa_qT, "raw0", writes=[b_raw[0]])
    p.dma("sync", raw[1][0:64, :], wa_kT, "raw1", writes=[b_raw[1]])
    p.dma("sync", raw[1][64:128, :], wa_kT, "raw1", writes=[b_raw[1]])
    load_v(wa_v, 64, 1)
    b_src = b_raw[0]
    qknorm(raw[0], 128, qn, b_qn, nr8[:, 2:3], True)
    b_src = b_raw[1]
    qknorm(raw[1], 128, kn, b_kn, nrt[:, 3:4], True)

    def wa_tiles(qp):
        kps, bts = [], []
        if qp > 0:
            kps.append(qp - 1); bts.append(0)
        kps.append(qp); bts.append(None)
        if qp < NQT - 1:
            kps.append(qp + 1); bts.append(2)
        return kps, bts

    attention(wa_tiles, lambda h: h * 64, lambda h, bt: wmask[:, bt * 128:(bt + 1) * 128], True, o_wa)
    return p.finish()


def k2a_inputs(b, j, ufm, utm, z, L):
    ts = slice(b * S, (b + 1) * S)
    cT, sT, pm = rope_tables_host()
    ident = np.eye(128, dtype=np.float32)
    bo = np.zeros((128, 128), np.float32); bo[0:64, 0:64] = 1; bo[64:, 64:] = 1
    pmm = np.zeros((128, 128), np.float32); pmm[0:64, 0:64] = pm; pmm[64:, 64:] = pm
    nrm = np.zeros((128, 4), np.float32)
    nrm[:, 0] = np.tile(z["na_q_norm"][L], 2); nrm[:, 1] = np.tile(z["na_k_norm"][L], 2)
    nrm[:, 2] = np.tile(z["wa_q_norm"][L], 2); nrm[:, 3] = np.tile(z["wa_k_norm"][L], 2)
    nb = na_bias_host(z["na_rpb"][L][2 * j:2 * j + 2])
    nb = np.ascontiguousarray(nb.transpose(2, 0, 1, 3)).reshape(128, 2 * 21 * 128)
    wm = np.ascontiguousarray(wa_mask_host().transpose(1, 0, 2)).reshape(128, 384)
    sink = np.broadcast_to(z["wa_sink"][L][2 * j:2 * j + 2][None, :], (128, 2)).copy()
    return {
        "na_qT": np.ascontiguousarray(ufm[(0 + j) * 128:(1 + j) * 128, ts]),
        "na_kT": np.ascontiguousarray(ufm[(4 + j) * 128:(5 + j) * 128, ts]),
        "na_v": np.ascontiguousarray(utm[ts, j * 128:(j + 1) * 128]),
        "wa_qT": np.ascontiguousarray(ufm[(8 + j) * 128:(9 + j) * 128, ts]),
        "wa_kT": np.ascontiguousarray(ufm[12 * 128 + (j // 2) * 64:12 * 128 + (j // 2 + 1) * 64, ts]),
        "wa_v": np.ascontiguousarray(utm[ts, 512 + (j // 2) * 64:512 + (j // 2 + 1) * 64]),
        "nrm": nrm, "na_bias": nb, "wa_mask": wm,
        "cst": np.concatenate([ident, bo, pmm], axis=1),
        "ropeT": np.concatenate([np.tile(cT, (2, 1)), np.tile(sT, (2, 1))], axis=1),
        "sink": sink,
    }


S = 8192
NCH = S // 128
NEG = -30000.0


def build_k2c():
    p = Prog()
    raw_in = p.dram("raw_in", [3, 128, S + 4], BF16, "ExternalInput")
    cw = p.dram("cw", [128, 15], F32, "ExternalInput")
    cb = p.dram("cb", [128, 3], F32, "ExternalInput")
    dt_in = p.dram("dt_in", [128, NCH * 4], F32, "ExternalInput")
    prm = p.dram("prm", [128, 10], F32, "ExternalInput")
    cst = p.dram("cst", [128, 6 * 128], F32, "ExternalInput")
    y_out = p.dram("y_out", [S, 128], F32, "ExternalOutput")

    cs = p.sb("cs", [128, 6 * 128], F32); b_cs = Buf("cs")
    U = cs[:, 0:128]; UT = cs[:, 128:256]; ones = cs[:, 256:384]; identf = cs[:, 384:512]
    mnf = cs[:, 512:640]; mnb = cs[:, 640:768]
    csb = p.sb("csb", [128, 6 * 128], BF16); b_csb = Buf("csb")
    Ub = csb[:, 0:128]; UTb = csb[:, 128:256]; onesb = csb[:, 256:384]; identb = csb[:, 384:512]
    mnfb = csb[:, 512:640]; mnbb = csb[:, 640:768]
    b_idb = b_csb
    dhf = p.sb("dhf", [128, NCH, 4], F32); dlf = p.sb("dlf", [128, NCH, 4], F32)
    dhb = p.sb("dhb", [128, NCH, 4], BF16); dlb = p.sb("dlb", [128, NCH, 4], BF16)
    cwt = p.sb("cwt", [128, 15], F32); b_cw = Buf("cw")
    cbt = p.sb("cbt", [128, 3], F32); b_cb = Buf("cb")
    prt = p.sb("prt", [128, 10], F32); b_pr = Buf("pr")
    acoef = p.sb("acoef", [128, 4], F32); b_ac = Buf("ac")
    diag = p.sb("diag", [128, 15, 128], BF16); b_dg = Buf("diag")
    raw = p.sb("raw", [128, S + 4], BF16); b_raw = Buf("raw")
    cT = [p.sb("cT%d" % i, [128, S], BF16) for i in range(2)]; b_cT = [Buf("cT0"), Buf("cT1")]
    x_tm = p.sb("x_tm", [128, NCH, 128], BF16); b_xtm = Buf("xtm")
    B_tm = p.sb("B_tm", [128, NCH, 128], BF16); b_btm = Buf("btm")
    dt = p.sb("dt", [128, NCH, 4], F32); b_dt = Buf("dt")
    dta = p.sb("dta", [128, NCH, 4], F32); b_dta = Buf("dta")
    yacc = p.sb("yacc", [128, NCH, 128], F32); b_y = Buf("yacc")

    psC = [p.ps("psC%d" % i, [128, 512]) for i in range(2)]; b_psC = [Buf("psC0"), Buf("psC1")]
    psT = p.ps("psT", [128, 256], BF16); b_psT = Buf("psT")
    psA = [p.ps("psA%d" % i, [128, 512]) for i in range(2)]; b_psA = [Buf("psA0"), Buf("psA1")]
    psS = p.ps("psS", [128, 512]); b_psS = [Buf("psS")] * 4
    psY = [p.ps("psY%d" % i, [128, 512]) for i in range(2)]; _by = [Buf("psY0"), Buf("psY1")]; b_psY = [_by[0], _by[0], _by[1], _by[1]]

    R = [[p.sb("R%d_%d" % (par, u), [128, 2, 128], BF16) for u in range(4)] for par in range(2)]
    b_R = [[Buf("R") for u in range(4)] for par in range(2)]
    DT_ = [[p.sb("D%d_%d" % (par, u), [128, 128], BF16) for u in range(4)] for par in range(2)]
    b_D = [[Buf("D") for u in range(4)] for par in range(2)]
    M = [[p.sb("M%d_%d" % (par, u), [128, 128], BF16) for u in range(4)] for par in range(2)]
    b_M = [[Buf("M") for u in range(4)] for par in range(2)]
    xw = [[p.sb("xw%d_%d" % (par, u), [128, 64], BF16) for u in range(4)] for par in range(2)]
    b_xw = [[Buf("xw") for u in range(4)] for par in range(2)]
    sm = [p.sb("sm%d" % par, [128, 8, 4], F32) for par in range(2)]
    b_sm = [Buf("sm0"), Buf("sm1")]
    yd = [[p.sb("yd%d_%d" % (par, u), [128, 64], F32) for u in range(4)] for par in range(2)]
    b_yd = [[Buf("yd") for u in range(4)] for par in range(2)]
    ytmp = [[p.sb("yt%d_%d" % (par, u), [128, 64], F32) for u in range(4)] for par in range(2)]
    b_yt = [[Buf("yt") for u in range(4)] for par in range(2)]
    hT = [p.sb("hT%d" % u, [128, 64], F32) for u in range(4)]; b_h = [Buf("h%d" % u) for u in range(4)]
    hb = [[p.sb("hb%d_%d" % (par, u), [128, 64], BF16) for u in range(4)] for par in range(2)]
    b_hb = [[Buf("hb") for u in range(4)] for par in range(2)]

    p.dma("sync", cs[:], cst, "cs", writes=[b_cs])
    p.dma("sync", cwt[:], cw, "cw", writes=[b_cw])
    p.dma("sync", cbt[:], cb, "cb", writes=[b_cb])
    p.dma("sync", prt[:], prm, "pr", writes=[b_pr])
    p.dma("sync", dt[:].rearrange("p c k -> p (c k)"), dt_in, "dt", writes=[b_dt])
    p.op("vector", lambda e: e.tensor_copy(out=csb[:], in_=cs[:]), reads=[b_cs], writes=[b_csb])
    for i in range(15):
        p.op("vector", lambda e, i=i: e.tensor_scalar(out=diag[:, i, :], in0=identf, scalar1=cwt[:, i:i + 1], scalar2=None,
                                                      op0=ALU.mult), reads=[b_cs, b_cw], writes=[b_dg])
    p.op("scalar", lambda e: e.activation(out=acoef[:], in_=prt[:, 4:8], func=AF.Exp), reads=[b_pr], writes=[b_ac])
    p.op("vector", lambda e: e.tensor_scalar(out=acoef[:], in0=acoef[:], scalar1=-1.0, scalar2=None, op0=ALU.mult),
         reads=[b_ac], writes=[b_ac])
    for k in range(4):
        p.op("vector", lambda e, k=k: e.tensor_scalar(out=dt[:, :, k], in0=dt[:, :, k], scalar1=prt[:, k:k + 1], scalar2=None,
                                                      op0=ALU.add), reads=[b_dt, b_pr], writes=[b_dt])
    p.op("scalar", lambda e: e.activation(out=dt[:], in_=dt[:], func=AF.Exp), reads=[b_dt], writes=[b_dt])
    p.op("scalar", lambda e: e.activation(out=dt[:], in_=dt[:], func=AF.Ln, bias=ones[:, 0:1], scale=1.0), reads=[b_dt, b_cs], writes=[b_dt])
    for k in range(4):
        p.op("vector", lambda e, k=k: e.tensor_scalar(out=dta[:, :, k], in0=dt[:, :, k], scalar1=acoef[:, k:k + 1], scalar2=None,
                                                      op0=ALU.mult), reads=[b_dt, b_ac], writes=[b_dta])

    p.op("vector", lambda e: e.tensor_copy(out=dhb[:], in_=dta[:]), reads=[b_dta], writes=[b_dta])
    p.op("vector", lambda e: e.tensor_copy(out=dhf[:], in_=dhb[:]), reads=[b_dta], writes=[b_dta])
    p.op("vector", lambda e: e.tensor_tensor(out=dlf[:], in0=dta[:], in1=dhf[:], op=ALU.subtract), reads=[b_dta], writes=[b_dta])
    p.op("vector", lambda e: e.tensor_copy(out=dlb[:], in_=dlf[:]), reads=[b_dta], writes=[b_dta])

    ci = 0
    for chunk, dst in ((0, 0), (1, 1), (2, 0)):
        p.dma("sync", raw[:], raw_in[chunk], "raw", writes=[b_raw])
        for tt in range(S // 512):
            b = ci % 2
            ci += 1
            for k in range(5):
                p.op("tensor", lambda e, b=b, k=k, tt=tt, chunk=chunk: e.matmul(
                    psC[b][:], lhsT=diag[:, chunk * 5 + k, :], rhs=raw[:, tt * 512 + k:tt * 512 + k + 512],
                    start=(k == 0), stop=(k == 4)), reads=[b_dg, b_raw], writes=[b_psC[b]], count=(k == 4))
            p.op("scalar", lambda e, b=b, tt=tt, chunk=chunk, dst=dst: e.activation(
                out=cT[dst][:, tt * 512:(tt + 1) * 512], in_=psC[b][:], func=AF.Silu, bias=cbt[:, chunk:chunk + 1]),
                reads=[b_psC[b], b_cb], writes=[b_cT[dst]])
        if chunk < 2:
            tgt, b_tgt = (x_tm, b_xtm) if chunk == 0 else (B_tm, b_btm)
            for c2 in range(NCH // 2):
                for q in range(2):
                    c = c2 * 2 + q
                    p.op("tensor", lambda e, c=c, q=q, dst=dst: e.transpose(psT[:, q * 128:(q + 1) * 128],
                                                                          cT[dst][:, c * 128:(c + 1) * 128], identb),
                         reads=[b_cT[dst], b_idb], writes=[b_psT], count=(q == 1))
                p.op("vector", lambda e, c2=c2, tgt=tgt: e.tensor_copy(
                    out=tgt[:, 2 * c2:2 * c2 + 2, :], in_=psT[:].rearrange("p (q f) -> p q f", q=2)),
                    reads=[b_psT], writes=[b_tgt])
    BcT = cT[1]; b_B = b_cT[1]
    CcT = cT[0]; b_C = b_cT[0]

    for h in range(2):
        p.op("vector", lambda e, h=h: e.tensor_scalar(out=yacc[:, :, h * 64:(h + 1) * 64], in0=x_tm[:, :, h * 64:(h + 1) * 64],
                                                      scalar1=prt[:, 8 + h:9 + h], scalar2=None, op0=ALU.mult),
             reads=[b_xtm, b_pr], writes=[b_y])
    for u in range(4):
        p.op("gpsimd", lambda e, u=u: e.memset(hT[u][:], 0.0), writes=[b_h[u]])
        p.op("gpsimd", lambda e, u=u: e.memset(hb[1][u][:], 0.0), writes=[b_hb[1][u]])

    def chunk_of(i, d):
        return i if d == 0 else NCH - 1 - i

    def front(i):
        par = i % 2
        A = psA[par]
        for d in range(2):
            c = chunk_of(i, d)
            p.op("tensor", lambda e, d=d, c=c, A=A: e.matmul(A[:, d * 128:(d + 1) * 128], lhsT=BcT[:, c * 128:(c + 1) * 128],
                                                             rhs=CcT[:, c * 128:(c + 1) * 128], start=True, stop=True),
                 reads=[b_B, b_C], writes=[b_psA[par]], count=False)
            for lhs, off in (((Ub if d == 0 else UTb), 256 + d * 8), (onesb, 260 + d * 8)):
                p.op("tensor", lambda e, c=c, A=A, lhs=lhs, off=off: e.matmul(A[:, off:off + 4], lhsT=lhs, rhs=dhb[:, c, :],
                                                                             start=True, stop=False),
                     reads=[b_csb, b_dta], writes=[b_psA[par]], count=False)
                p.op("tensor", lambda e, c=c, A=A, lhs=lhs, off=off: e.matmul(A[:, off:off + 4], lhsT=lhs, rhs=dlb[:, c, :],
                                                                             start=False, stop=True),
                     reads=[b_csb, b_dta], writes=[b_psA[par]], count=(d == 1 and off == 268))
        s_ = sm[par]
        for d in range(2):
            ks = slice(d * 2, d * 2 + 2)
            ac = A[:, 256 + d * 8 + d * 2:256 + d * 8 + d * 2 + 2]
            tt_ = A[:, 260 + d * 8 + d * 2:260 + d * 8 + d * 2 + 2]
            p.op("vector", lambda e, ac=ac, ks=ks: e.tensor_scalar(out=s_[:, 0, ks], in0=ac, scalar1=-1.0, scalar2=None, op0=ALU.mult),
                 reads=[b_psA[par]], writes=[b_sm[par]])
            p.op("vector", lambda e, ac=ac, tt_=tt_, ks=ks: e.tensor_tensor(out=s_[:, 1, ks], in0=tt_, in1=s_[:, 0, ks], op=ALU.add),
                 reads=[b_psA[par], b_sm[par]], writes=[b_sm[par]])
            p.op("scalar", lambda e, tt_=tt_, ks=ks: e.activation(out=s_[:, 3, ks], in_=tt_, func=AF.Exp),
                 reads=[b_psA[par]], writes=[b_sm[par]])
            p.op("scalar", lambda e, ac=ac, ks=ks: e.activation(out=s_[:, 4, ks], in_=ac, func=AF.Exp),
                 reads=[b_psA[par]], writes=[b_sm[par]])
        p.op("scalar", lambda e: e.activation(out=s_[:, 2, :], in_=s_[:, 1, :], func=AF.Exp), reads=[b_sm[par]], writes=[b_sm[par]])
        for d in range(2):
            c = chunk_of(i, d)
            p.op("vector", lambda e, d=d, c=c: e.tensor_tensor(out=s_[:, 5, d * 2:d * 2 + 2], in0=s_[:, 2, d * 2:d * 2 + 2],
                                                               in1=dt[:, c, d * 2:d * 2 + 2], op=ALU.mult),
                 reads=[b_sm[par], b_dt], writes=[b_sm[par]])
        for u in range(4):
            d, h = u // 2, u % 2
            c = chunk_of(i, d)
            for q, src in ((0, dhf), (1, dlf)):
                p.op("gpsimd", lambda e, u=u, d=d, c=c, q=q, src=src: e.tensor_scalar(
                    out=R[par][u][:, q, :], in0=(U if d == 0 else UT), scalar1=src[:, c, u:u + 1], scalar2=None, op0=ALU.mult),
                    reads=[b_cs, b_dta], writes=[b_R[par][u]])
        for u in range(4):
            d = u // 2
            p.op("tensor", lambda e, u=u: e.matmul(psS[:, u * 128:(u + 1) * 128], lhsT=onesb, rhs=R[par][u][:, 0, :], start=True, stop=False),
                 reads=[b_csb, b_R[par][u]], writes=[b_psS[u]], count=False)
            p.op("tensor", lambda e, u=u: e.matmul(psS[:, u * 128:(u + 1) * 128], lhsT=onesb, rhs=R[par][u][:, 1, :], start=False, stop=False),
                 reads=[b_csb, b_R[par][u]], writes=[b_psS[u]], count=False)
            p.op("tensor", lambda e, u=u, d=d: e.matmul(psS[:, u * 128:(u + 1) * 128], lhsT=identb, rhs=(mnfb if d == 0 else mnbb),
                                                        start=False, stop=True),
                 reads=[b_csb], writes=[b_psS[u]], count=(u == 3))
        for u in range(4):
            p.op("scalar", lambda e, u=u: e.activation(out=DT_[par][u][:], in_=psS[:, u * 128:(u + 1) * 128], func=AF.Exp,
                                                       bias=s_[:, 0, u:u + 1]),
                 reads=[b_psS[u], b_sm[par]], writes=[b_D[par][u]])
        for u in range(4):
            d, h = u // 2, u % 2
            c = chunk_of(i, d)
            p.op("vector", lambda e, u=u, d=d, c=c, A=A: e.scalar_tensor_tensor(
                out=M[par][u][:], in0=A[:, d * 128:(d + 1) * 128], scalar=dt[:, c, u:u + 1], in1=DT_[par][u][:],
                op0=ALU.mult, op1=ALU.mult), reads=[b_psA[par], b_dt, b_D[par][u]], writes=[b_M[par][u]])
            p.op("vector", lambda e, u=u, h=h, c=c: e.tensor_scalar(out=xw[par][u][:], in0=x_tm[:, c, h * 64:(h + 1) * 64],
                                                                    scalar1=s_[:, 5, u:u + 1], scalar2=None, op0=ALU.mult),
                 reads=[b_xtm, b_sm[par]], writes=[b_xw[par][u]])

    def back(i):
        par = i % 2
        s_ = sm[par]
        for u in range(4):
            d, h = u // 2, u % 2
            c = chunk_of(i, d)
            Y = psY[u // 2]
            o = (u % 2) * 192
            p.op("tensor", lambda e, u=u, h=h, c=c, Y=Y, o=o: e.matmul(Y[:, o:o + 64], lhsT=M[par][u][:],
                                                                      rhs=x_tm[:, c, h * 64:(h + 1) * 64], start=True, stop=True),
                 reads=[b_M[par][u], b_xtm], writes=[b_psY[u]], count=False)
            p.op("tensor", lambda e, u=u, c=c, Y=Y, o=o: e.matmul(Y[:, o + 64:o + 128], lhsT=CcT[:, c * 128:(c + 1) * 128],
                                                                 rhs=hb[1 - par][u][:], start=True, stop=True),
                 reads=[b_C, b_hb[1 - par][u]], writes=[b_psY[u]], count=False)
            p.op("tensor", lambda e, u=u, c=c, Y=Y, o=o: e.matmul(Y[:, o + 128:o + 192], lhsT=B_tm[:, c, :],
                                                                 rhs=xw[par][u][:], start=True, stop=True),
                 reads=[b_btm, b_xw[par][u]], writes=[b_psY[u]], count=(u % 2 == 1))
        for u in range(4):
            d, h = u // 2, u % 2
            c = chunk_of(i, d)
            Y = psY[u // 2]
            o = (u % 2) * 192
            p.op("vector", lambda e, u=u, Y=Y, o=o: e.scalar_tensor_tensor(out=hT[u][:], in0=hT[u][:], scalar=s_[:, 3, u:u + 1],
                                                                          in1=Y[:, o + 128:o + 192], op0=ALU.mult, op1=ALU.add),
                 reads=[b_h[u], b_sm[par], b_psY[u]], writes=[b_h[u]])
            p.op("scalar", lambda e, u=u: e.copy(out=hb[par][u][:], in_=hT[u][:]), reads=[b_h[u]], writes=[b_hb[par][u]])
            p.op("scalar", lambda e, u=u, Y=Y, o=o: e.copy(out=yd[par][u][:], in_=Y[:, o:o + 64]),
                 reads=[b_psY[u]], writes=[b_yd[par][u]])
            p.op("vector", lambda e, u=u, Y=Y, o=o: e.scalar_tensor_tensor(out=ytmp[par][u][:], in0=Y[:, o + 64:o + 128],
                                                                          scalar=s_[:, 4, u:u + 1], in1=yd[par][u][:],
                                                                          op0=ALU.mult, op1=ALU.add),
                 reads=[b_psY[u], b_sm[par], b_yd[par][u]], writes=[b_yt[par][u]])
            p.op("gpsimd", lambda e, u=u, h=h, c=c: e.tensor_tensor(out=yacc[:, c, h * 64:(h + 1) * 64],
                                                                    in0=yacc[:, c, h * 64:(h + 1) * 64], in1=ytmp[par][u][:],
                                                                    op=ALU.add),
                 reads=[b_yt[par][u], b_y], writes=[b_y])

    front(0)
    for i in range(NCH):
        if i + 1 < NCH:
            front(i + 1)
        back(i)
    p.dma("sync", y_out.rearrange("(c l) f -> l c f", l=128), yacc[:], "yout", reads=[b_y])
    return p.finish()


def k2c_consts():
    U = np.triu(np.ones((128, 128), np.float32))
    UT = np.tril(np.ones((128, 128), np.float32))
    ones = np.ones((128, 128), np.float32)
    ident = np.eye(128, dtype=np.float32)
    mnf = (1 - U) * NEG
    mnb = (1 - UT) * NEG
    return np.concatenate([U, UT, ones, ident, mnf, mnb], axis=1).astype(np.float32)


def k2c_inputs(b, j, ufm, udt, z, L):
    ts = slice(b * S, (b + 1) * S)
    g = j // 2
    XBC0 = (8 + 5 + 4) * 128
    rows = [XBC0 + j * 128, XBC0 + 512 + g * 128, XBC0 + 768 + g * 128]
    raw = np.zeros((3, 128, S + 4), NPBF)
    for i, r0 in enumerate(rows):
        raw[i, :, 2:S + 2] = ufm[r0:r0 + 128, ts]
    cwf = z["ssm_conv_w"][L]
    cbf = z["ssm_conv_b"][L]
    chs = [slice(j * 128, (j + 1) * 128), slice(512 + g * 128, 512 + (g + 1) * 128), slice(768 + g * 128, 768 + (g + 1) * 128)]
    cw = np.concatenate([cwf[:, ch].T for ch in chs], axis=1)
    cb = np.stack([cbf[ch] for ch in chs], axis=1)
    cols = [0 * 8 + 2 * j, 0 * 8 + 2 * j + 1, 1 * 8 + 2 * j, 1 * 8 + 2 * j + 1]
    d4 = udt[ts][:, cols]
    dt_in = np.ascontiguousarray(d4.reshape(NCH, 128, 4).transpose(1, 0, 2)).reshape(128, NCH * 4)
    prm = np.zeros((128, 10), np.float32)
    prm[:, 0:4] = z["ssm_dt_bias"][L].reshape(16)[cols][None, :]
    prm[:, 4:8] = z["ssm_a_log"][L].reshape(16)[cols][None, :]
    prm[:, 8:10] = z["ssm_d"][L][2 * j:2 * j + 2][None, :]
    return {"raw_in": raw, "cw": np.ascontiguousarray(cw), "cb": np.ascontiguousarray(cb), "dt_in": dt_in, "prm": prm,
            "cst": k2c_consts()}


D = 2048
KC = 16
EPS = 1e-6
TT = 256


def build_k3(T=2048):
    p = Prog()
    NT = T // TT
    xT = p.dram("xT", [D, T], F32, "ExternalInput")
    oabT = p.dram("oabT", [1024, T], BF16, "ExternalInput")
    yT = p.dram("yT", [512, T], F32, "ExternalInput")
    zT = p.dram("zT", [512, T], BF16, "ExternalInput")
    confT = p.dram("confT", [1024, T + 30], BF16, "ExternalInput")
    prm = p.dram("prm", [128, 4 * 5 + 16], F32, "ExternalInput")
    dww = p.dram("dww", [128, 4 * 31], F32, "ExternalInput")
    w_out = p.dram("w_out", [D, D], F32, "ExternalInput")
    w_r = p.dram("w_r", [128, KC * 16], F32, "ExternalInput")
    identd = p.dram("identd", [128, 128], F32, "ExternalInput")
    x1T = p.dram("x1T", [D, T], F32, "ExternalOutput")
    hT = p.dram("hT", [D, T], BF16, "ExternalOutput")
    aff = p.dram("aff", [T, 16], F32, "ExternalOutput")

    wo = p.sb("wo", [128, KC, D], BF16); b_wo = Buf("wo")
    prt = p.sb("prt", [128, 36], F32); b_pr = Buf("pr")
    dwt = p.sb("dwt", [128, 124], F32); b_dw = Buf("dw")
    idf = p.sb("idf", [128, 128], F32); b_id = Buf("id")
    diag = p.sb("diag", [128, 124, 128], BF16); b_dg = Buf("dg")
    ones = p.sb("ones", [128, 128], BF16); b_on = Buf("ones")
    epst = p.sb("epst", [128, 1], F32); b_eps = Buf("eps")
    wrf = p.sb("wrf", [128, KC * 16], F32); b_wr = Buf("wr")
    wrh = p.sb("wrh", [128, KC * 16], BF16)
    wrhf = p.sb("wrhf", [128, KC * 16], F32)
    wrl = p.sb("wrl", [128, KC * 16], BF16)

    xt = p.sb("xt", [128, KC, TT], F32); b_xt = Buf("xt")
    mix = p.sb("mix", [128, KC, TT], BF16); b_mix = [Buf("mix%d" % i) for i in range(4)]
    yt = p.sb("yt", [128, 4, TT], F32); b_yt = Buf("yt")
    zt = p.sb("zt", [128, 4, TT], BF16); b_zt = Buf("zt")
    gt = p.sb("gt", [128, 4, TT], F32); b_gt = Buf("gt")
    sq = p.sb("sq", [128, KC, TT], BF16); b_sq = Buf("sq")
    rstd = p.sb("rstd", [128, TT], F32); b_rstd = Buf("rstd")
    cf = p.sb("cf", [128, 8, TT + 30], BF16); b_cf = Buf("cf")
    sg = p.sb("sg", [128, 4, TT + 30], BF16); b_sg = Buf("sg")
    glu = p.sb("glu", [128, 4, TT + 30], BF16); b_glu = Buf("glu")
    cv = p.sb("cv", [128, 4, TT], F32); b_cv = Buf("cv")
    cvb = p.sb("cvb", [128, 4, TT], BF16); b_cvb = Buf("cvb")
    mean = p.sb("mean", [128, TT], F32); b_mean = Buf("mean")
    var = p.sb("var", [128, TT], F32); b_var = Buf("var")
    t1 = p.sb("t1", [128, 4, TT], F32); b_t1 = Buf("t1")
    hf = p.sb("hf", [128, KC, TT], F32); b_hf = Buf("hf")
    hh = p.sb("hh", [128, KC, TT], BF16); b_hh = Buf("hh")
    hl = p.sb("hl", [128, KC, TT], BF16); b_hl = Buf("hl")
    lg = p.sb("lg", [128, 2, 16], F32); b_lg = Buf("lg")
    smx = p.sb("smx", [128, 2, 4], F32); b_smx = Buf("smx")

    psn = p.ps("psn", [128, 512]); b_psn = Buf("psn")
    psc = [p.ps("psc%d" % i, [128, 512]) for i in range(2)]; b_psc = [Buf("psc0"), Buf("psc1")]
    psm = [p.ps("psm%d" % i, [128, 512]) for i in range(3)]; b_psm = [Buf("psm%d" % i) for i in range(3)]
    psr = p.ps("psr", [128, 512]); b_psr = Buf("psr")

    p.dma("sync", prt[:], prm, "pr", writes=[b_pr])
    p.dma("sync", dwt[:], dww, "dw", writes=[b_dw])
    p.dma("sync", idf[:], identd, "id", writes=[b_id])
    p.dma("sync", wrf[:], w_r, "wr", writes=[b_wr])
    p.op("gpsimd", lambda e: e.memset(ones[:], 1.0), writes=[b_on])
    p.op("gpsimd", lambda e: e.memset(epst[:], EPS), writes=[b_eps])
    w_v = w_out.rearrange("(k p) n -> p k n", p=128)
    for q in range(4):
        p.dma("gpsimd", wo[:, :, q * 512:(q + 1) * 512], w_v[:, :, q * 512:(q + 1) * 512], "wo", writes=[b_wo])
    for i in range(124):
        p.op("vector", lambda e, i=i: e.tensor_scalar(out=diag[:, i, :], in0=idf[:], scalar1=dwt[:, i:i + 1], scalar2=None,
                                                      op0=ALU.mult), reads=[b_id, b_dw], writes=[b_dg])
    p.op("vector", lambda e: e.tensor_copy(out=wrh[:], in_=wrf[:]), reads=[b_wr], writes=[b_wr])
    p.op("vector", lambda e: e.tensor_copy(out=wrhf[:], in_=wrh[:]), reads=[b_wr], writes=[b_wr])
    p.op("vector", lambda e: e.tensor_tensor(out=wrhf[:], in0=wrf[:], in1=wrhf[:], op=ALU.subtract), reads=[b_wr], writes=[b_wr])
    p.op("vector", lambda e: e.tensor_copy(out=wrl[:], in_=wrhf[:]), reads=[b_wr], writes=[b_wr])

    xT_v = xT.rearrange("(k p) t -> p k t", p=128)
    oab_v = oabT.rearrange("(k p) t -> p k t", p=128)
    yT_v = yT.rearrange("(k p) t -> p k t", p=128)
    zT_v = zT.rearrange("(k p) t -> p k t", p=128)
    cf_v = confT.rearrange("(k p) t -> p k t", p=128)
    x1_v = x1T.rearrange("(k p) t -> p k t", p=128)
    hT_v = hT.rearrange("(k p) t -> p k t", p=128)

    mi = 0
    for tt in range(NT):
        ts = slice(tt * TT, (tt + 1) * TT)
        p.dma("sync", xt[:], xT_v[:, :, ts], "xt", writes=[b_xt])
        p.dma("sync", mix[:, 0:8, :], oab_v[:, :, ts], "mix", writes=[b_mix[0], b_mix[1]])
        p.dma("sync", yt[:], yT_v[:, :, ts], "yt", writes=[b_yt])
        p.dma("sync", zt[:], zT_v[:, :, ts], "zt", writes=[b_zt])
        p.dma("sync", cf[:], cf_v[:, :, tt * TT:tt * TT + TT + 30], "cf", writes=[b_cf])

        p.op("scalar", lambda e: e.activation(out=gt[:], in_=zt[:], func=AF.Silu), reads=[b_zt], writes=[b_gt])
        p.op("vector", lambda e: e.tensor_tensor(out=gt[:], in0=gt[:], in1=yt[:], op=ALU.mult), reads=[b_gt, b_yt], writes=[b_gt])
        p.op("scalar", lambda e: e.activation(out=sq[:, 0:4, :], in_=gt[:], func=AF.Square), reads=[b_gt], writes=[b_sq])
        for k in range(4):
            p.op("tensor", lambda e, k=k: e.matmul(psn[:, 0:TT], lhsT=ones[:], rhs=sq[:, k, :], start=(k == 0), stop=(k == 3)),
                 reads=[b_on, b_sq], writes=[b_psn], count=(k == 3))
        p.op("scalar", lambda e: e.activation(out=rstd[:], in_=psn[:, 0:TT], func=AF.Sqrt, bias=epst[:], scale=1.0 / 512),
             reads=[b_psn, b_eps], writes=[b_rstd])
        p.op("vector", lambda e: e.reciprocal(out=rstd[:], in_=rstd[:]), reads=[b_rstd], writes=[b_rstd])
        for k in range(4):
            p.op("vector", lambda e, k=k: e.scalar_tensor_tensor(out=mix[:, 8 + k, :], in0=gt[:, k, :], scalar=prt[:, k:k + 1],
                                                                 in1=rstd[:], op0=ALU.mult, op1=ALU.mult),
                 reads=[b_gt, b_pr, b_rstd], writes=[b_mix[2]])

        p.op("scalar", lambda e: e.activation(out=sg[:], in_=cf[:, 4:8, :], func=AF.Sigmoid), reads=[b_cf], writes=[b_sg])
        p.op("vector", lambda e: e.tensor_tensor(out=glu[:], in0=cf[:, 0:4, :], in1=sg[:], op=ALU.mult),
             reads=[b_cf, b_sg], writes=[b_glu])
        for k in range(4):
            b = k % 2
            for j in range(31):
                p.op("tensor", lambda e, k=k, j=j, b=b: e.matmul(psc[b][:, 0:TT], lhsT=diag[:, k * 31 + j, :],
                                                                 rhs=glu[:, k, j:j + TT], start=(j == 0), stop=(j == 30)),
                     reads=[b_dg, b_glu], writes=[b_psc[b]], count=(j == 30))
            p.op("scalar", lambda e, k=k, b=b: e.activation(out=cv[:, k, :], in_=psc[b][:, 0:TT], func=AF.Identity,
                                                            bias=prt[:, 4 + k:5 + k]),
                 reads=[b_psc[b], b_pr], writes=[b_cv])
        p.op("vector", lambda e: e.tensor_copy(out=cvb[:], in_=cv[:]), reads=[b_cv], writes=[b_cvb])
        p.op("scalar", lambda e: e.activation(out=sq[:, 4:8, :], in_=cv[:], func=AF.Square), reads=[b_cv], writes=[b_sq])
        for k in range(4):
            p.op("tensor", lambda e, k=k: e.matmul(psn[:, 0:TT], lhsT=ones[:], rhs=cvb[:, k, :], start=(k == 0), stop=(k == 3)),
                 reads=[b_on, b_cvb], writes=[b_psn], count=False)
        for k in range(4):
            p.op("tensor", lambda e, k=k: e.matmul(psn[:, TT:2 * TT], lhsT=ones[:], rhs=sq[:, 4 + k, :], start=(k == 0), stop=(k == 3)),
                 reads=[b_on, b_sq], writes=[b_psn], count=(k == 3))
        p.op("vector", lambda e: e.tensor_scalar(out=mean[:], in0=psn[:, 0:TT], scalar1=1.0 / 512, scalar2=None, op0=ALU.mult),
             reads=[b_psn], writes=[b_mean])
        p.op("vector", lambda e: e.tensor_tensor(out=var[:], in0=mean[:], in1=mean[:], op=ALU.mult), reads=[b_mean], writes=[b_var])
        p.op("vector", lambda e: e.scalar_tensor_tensor(out=var[:], in0=psn[:, TT:2 * TT], scalar=1.0 / 512, in1=var[:],
                                                        op0=ALU.mult, op1=ALU.subtract),
             reads=[b_psn, b_var], writes=[b_var])
        p.op("scalar", lambda e: e.activation(out=var[:], in_=var[:], func=AF.Sqrt, bias=epst[:], scale=1.0),
             reads=[b_var, b_eps], writes=[b_var])
        p.op("vector", lambda e: e.reciprocal(out=var[:], in_=var[:]), reads=[b_var], writes=[b_var])
        for k in range(4):
            p.op("vector", lambda e, k=k: e.tensor_tensor(out=t1[:, k, :], in0=cv[:, k, :], in1=mean[:], op=ALU.subtract),
                 reads=[b_cv, b_mean], writes=[b_t1])
            p.op("vector", lambda e, k=k: e.tensor_tensor(out=t1[:, k, :], in0=t1[:, k, :], in1=var[:], op=ALU.mult),
                 reads=[b_t1, b_var], writes=[b_t1])
            p.op("scalar", lambda e, k=k: e.activation(out=mix[:, 12 + k, :], in_=t1[:, k, :], func=AF.Silu,
                                                       bias=prt[:, 12 + k:13 + k], scale=prt[:, 8 + k:9 + k]),
                 reads=[b_t1, b_pr], writes=[b_mix[3]])

        for dc in range(KC):
            b = mi % 3
            mi += 1
            for k in range(KC):
                p.op("tensor", lambda e, dc=dc, k=k, b=b: e.matmul(psm[b][:, 0:TT], lhsT=wo[:, k, dc * 128:(dc + 1) * 128],
                                                                   rhs=mix[:, k, :], start=(k == 0), stop=(k == KC - 1)),
                     reads=[b_wo] + b_mix, writes=[b_psm[b]], count=(k == KC - 1))
            p.op("vector", lambda e, dc=dc, b=b: e.tensor_tensor(out=xt[:, dc, :], in0=xt[:, dc, :], in1=psm[b][:, 0:TT], op=ALU.add),
                 reads=[b_psm[b], b_xt], writes=[b_xt])
        p.dma("sync", x1_v[:, :, ts], xt[:], "x1o", reads=[b_xt])

        p.op("scalar", lambda e: e.activation(out=sq[:], in_=xt[:], func=AF.Square), reads=[b_xt], writes=[b_sq])
        for k in range(KC):
            p.op("tensor", lambda e, k=k: e.matmul(psn[:, 0:TT], lhsT=ones[:], rhs=sq[:, k, :], start=(k == 0), stop=(k == KC - 1)),
                 reads=[b_on, b_sq], writes=[b_psn], count=(k == KC - 1))
        p.op("scalar", lambda e: e.activation(out=rstd[:], in_=psn[:, 0:TT], func=AF.Sqrt, bias=epst[:], scale=1.0 / D),
             reads=[b_psn, b_eps], writes=[b_rstd])
        p.op("vector", lambda e: e.reciprocal(out=rstd[:], in_=rstd[:]), reads=[b_rstd], writes=[b_rstd])
        for k in range(KC):
            p.op("vector", lambda e, k=k: e.scalar_tensor_tensor(out=hf[:, k, :], in0=xt[:, k, :], scalar=prt[:, 20 + k:21 + k],
                                                                 in1=rstd[:], op0=ALU.mult, op1=ALU.mult),
                 reads=[b_xt, b_pr, b_rstd], writes=[b_hf])
        p.op("scalar", lambda e: e.copy(out=hh[:], in_=hf[:]), reads=[b_hf], writes=[b_hh])
        p.op("gpsimd", lambda e: e.tensor_tensor(out=hf[:], in0=hf[:], in1=hh[:], op=ALU.subtract), reads=[b_hf, b_hh], writes=[b_hf])
        p.op("scalar", lambda e: e.copy(out=hl[:], in_=hf[:]), reads=[b_hf], writes=[b_hl])
        p.dma("sync", hT_v[:, :, ts], hh[:], "ho", reads=[b_hh])
        for s2 in range(TT // 128):
            first = True
            for k in range(KC):
                for (a_, w_) in ((hh, wrh), (hh, wrl), (hl, wrh)):
                    last = (k == KC - 1 and a_ is hl)
                    p.op("tensor", lambda e, s2=s2, k=k, a_=a_, w_=w_, first=first, last=last: e.matmul(
                        psr[:, s2 * 16:(s2 + 1) * 16], lhsT=a_[:, k, s2 * 128:(s2 + 1) * 128], rhs=w_[:, k * 16:(k + 1) * 16],
                        start=first, stop=last), reads=[b_hh, b_hl, b_wr], writes=[b_psr], count=(last and s2 == TT // 128 - 1))
                    first = False
        p.op("vector", lambda e: e.tensor_copy(out=lg[:], in_=psr[:, 0:32].rearrange("p (s e) -> p s e", s=2)),
             reads=[b_psr], writes=[b_lg])
        for s2 in range(TT // 128):
            p.op("vector", lambda e, s2=s2: e.reduce_max(out=smx[:, s2, 0:1], in_=lg[:, s2, :], axis=AX.X), reads=[b_lg], writes=[b_smx])
            p.op("vector", lambda e, s2=s2: e.tensor_scalar(out=smx[:, s2, 1:2], in0=smx[:, s2, 0:1], scalar1=-1.0, scalar2=None,
                                                            op0=ALU.mult), reads=[b_smx], writes=[b_smx])
            p.op("scalar", lambda e, s2=s2: e.activation(out=lg[:, s2, :], in_=lg[:, s2, :], func=AF.Exp, bias=smx[:, s2, 1:2]),
                 reads=[b_lg, b_smx], writes=[b_lg])
            p.op("vector", lambda e, s2=s2: e.reduce_sum(out=smx[:, s2, 2:3], in_=lg[:, s2, :], axis=AX.X), reads=[b_lg], writes=[b_smx])
            p.op("vector", lambda e, s2=s2: e.reciprocal(out=smx[:, s2, 3:4], in_=smx[:, s2, 2:3]), reads=[b_smx], writes=[b_smx])
            p.op("vector", lambda e, s2=s2: e.tensor_scalar(out=lg[:, s2, :], in0=lg[:, s2, :], scalar1=smx[:, s2, 3:4], scalar2=None,
                                                            op0=ALU.mult), reads=[b_lg, b_smx], writes=[b_lg])
        p.dma("sync", aff[tt * TT:(tt + 1) * TT, :].rearrange("(s p) e -> p s e", p=128), lg[:], "affo", reads=[b_lg])
    return p.finish()


def k3_inputs(c, x_tm, o_na, o_wa, y, ufm, z, L):
    T = 2048
    ts = slice(c * T, (c + 1) * T)
    b = c // 4
    Z0 = (8 + 5) * 128
    CF0 = (8 + 5 + 12) * 128
    conf = np.zeros((1024, T + 30), NPBF)
    lo = c * T - 15
    hi = c * T + T + 15
    slo = max(lo, b * 8192)
    shi = min(hi, (b + 1) * 8192)
    conf[:, slo - lo:slo - lo + (shi - slo)] = ufm[CF0:CF0 + 1024, slo:shi]
    prm = np.zeros((128, 36), np.float32)
    prm[:, 0:4] = z["ssm_norm_w"][L].reshape(4, 128).T
    prm[:, 4:8] = z["conf_dw_b"][L].reshape(4, 128).T
    prm[:, 8:12] = z["conf_ln_w"][L].reshape(4, 128).T
    prm[:, 12:16] = z["conf_ln_b"][L].reshape(4, 128).T
    prm[:, 20:36] = z["ffn_norm_w"][L].reshape(16, 128).T
    dww = np.ascontiguousarray(z["conf_dw_w"][L].reshape(31, 4, 128).transpose(2, 1, 0)).reshape(128, 124)
    w_r = np.ascontiguousarray(z["w_router"][L].reshape(16, 128, 16).transpose(1, 0, 2)).reshape(128, 256)
    return {
        "xT": np.ascontiguousarray(x_tm[ts].T),
        "oabT": np.ascontiguousarray(np.concatenate([o_na[ts], o_wa[ts]], axis=1).T),
        "yT": np.ascontiguousarray(y[ts].T),
        "zT": np.ascontiguousarray(ufm[Z0:Z0 + 512, ts]),
        "confT": conf, "prm": prm, "dww": dww, "w_out": z["w_out"][L], "w_r": w_r,
        "identd": np.eye(128, dtype=np.float32),
    }


D = 2048
FF = 1024
NIT = 40


def build_k4(S=8192, CAP=1024):
    p = Prog()
    NJ = S // 128
    NSH = CAP // 512
    affp = p.dram("affp", [128, 4 * NJ], F32, "ExternalInput")
    h_tm = p.dram("h_tm", [2 * S, D], BF16, "ExternalInput")
    wg = p.dram("wg", [2, D, FF], F32, "ExternalInput")
    wu = p.dram("wu", [2, D, FF], F32, "ExternalInput")
    wd = p.dram("wd", [2, FF, D], F32, "ExternalInput")
    cst = p.dram("cst", [128, 128 + CAP], F32, "ExternalInput")
    y_out = p.dram("y_out", [4, CAP, D], BF16, "ExternalOutput")
    slot_out = p.dram("slot_out", [128, 4 * NJ], F32, "ExternalOutput")

    csf = p.sb("csf", [128, 128 + CAP], F32); b_cs = Buf("cs")
    iota = csf[:, 128:128 + CAP]
    utb = p.sb("utb", [128, 128], BF16); b_ut = Buf("ut")
    onesb = p.sb("onesb", [128, 128], BF16); b_on = Buf("ones")
    aff3 = p.sb("aff3", [128, 4, NJ], F32); b_aff = Buf("aff")
    cmp_ = p.sb("cmp", [128, 4, NJ], F32); b_cmp = Buf("cmp")
    cmpb = p.sb("cmpb", [128, 4, NJ], BF16); b_cmpb = Buf("cmpb")
    sc1 = p.sb("sc1", [128, 4, NJ], F32); b_sc1 = Buf("sc1")
    sc2 = p.sb("sc2", [128, 4, NJ], F32); b_sc2 = Buf("sc2")
    slotc = p.sb("slotc", [128, 4, NJ], F32); b_slot = Buf("slot")
    sm = p.sb("sm", [128, 8, 4], F32); b_sm = Buf("sm")
    cntb = p.sb("cntb", [128, 4], BF16); b_cntb = Buf("cntb")
    ps = [p.ps("ps%d" % i, [128, 512]) for i in range(8)]; b_ps = [Buf("ps%d" % i) for i in range(8)]

    p.dma("sync", csf[:], cst, "cs", writes=[b_cs])
    p.dma("sync", aff3[:].rearrange("p i j -> p (i j)"), affp, "aff", writes=[b_aff])
    p.op("vector", lambda e: e.tensor_copy(out=utb[:], in_=csf[:, 0:128]), reads=[b_cs], writes=[b_ut])
    p.op("gpsimd", lambda e: e.memset(onesb[:], 1.0), writes=[b_on])
    p.op("gpsimd", lambda e: e.memset(sm[:], 0.0), writes=[b_sm])
    p.op("gpsimd", lambda e: e.memset(sm[:, 1, :], 1.0), reads=[b_sm], writes=[b_sm])

    lo = sm[:, 0, :]; hi = sm[:, 1, :]; mid = sm[:, 2, :]; cpart = sm[:, 3, :]; ge = sm[:, 4, :]; dd = sm[:, 5, :]
    V = "vector"

    def count_ge(thr):
        for i in range(4):
            p.op(V, lambda e, i=i: e.tensor_scalar(out=cmp_[:, i, :], in0=aff3[:, i, :], scalar1=thr[:, i:i + 1], scalar2=None,
                                                   op0=ALU.is_ge), reads=[b_aff, b_sm], writes=[b_cmp])

    for it in range(NIT):
        p.op(V, lambda e: e.tensor_tensor(out=mid, in0=lo, in1=hi, op=ALU.add), reads=[b_sm], writes=[b_sm])
        p.op(V, lambda e: e.tensor_scalar(out=mid, in0=mid, scalar1=0.5, scalar2=None, op0=ALU.mult), reads=[b_sm], writes=[b_sm])
        count_ge(mid)
        p.op(V, lambda e: e.reduce_sum(out=cpart, in_=cmp_[:], axis=AX.X), reads=[b_cmp], writes=[b_sm])
        p.op(V, lambda e: e.tensor_copy(out=cntb[:], in_=cpart), reads=[b_sm], writes=[b_cntb])
        p.op("tensor", lambda e: e.matmul(ps[0][:, 0:4], lhsT=onesb[:], rhs=cntb[:], start=True, stop=True),
             reads=[b_on, b_cntb], writes=[b_ps[0]])
        p.op(V, lambda e: e.tensor_scalar(out=ge, in0=ps[0][:, 0:4], scalar1=float(CAP), scalar2=None, op0=ALU.is_ge),
             reads=[b_ps[0]], writes=[b_sm])
        p.op(V, lambda e: e.tensor_tensor(out=dd, in0=mid, in1=lo, op=ALU.subtract), reads=[b_sm], writes=[b_sm])
        p.op(V, lambda e: e.tensor_tensor(out=dd, in0=dd, in1=ge, op=ALU.mult), reads=[b_sm], writes=[b_sm])
        p.op(V, lambda e: e.tensor_tensor(out=lo, in0=lo, in1=dd, op=ALU.add), reads=[b_sm], writes=[b_sm])
        p.op(V, lambda e: e.tensor_tensor(out=dd, in0=hi, in1=mid, op=ALU.subtract), reads=[b_sm], writes=[b_sm])
        p.op(V, lambda e: e.tensor_tensor(out=dd, in0=dd, in1=ge, op=ALU.mult), reads=[b_sm], writes=[b_sm])
        p.op(V, lambda e: e.tensor_tensor(out=hi, in0=mid, in1=dd, op=ALU.add), reads=[b_sm], writes=[b_sm])

    count_ge(lo)
    p.op(V, lambda e: e.tensor_copy(out=cmpb[:], in_=cmp_[:]), reads=[b_cmp], writes=[b_cmpb])
    cflat = cmpb[:].rearrange("p i j -> p (i j)")
    p.op("tensor", lambda e: e.matmul(ps[1][:, 0:4 * NJ], lhsT=utb[:], rhs=cflat, start=True, stop=True),
         reads=[b_ut, b_cmpb], writes=[b_ps[1]])
    p.op("tensor", lambda e: e.matmul(ps[2][:, 0:4 * NJ], lhsT=onesb[:], rhs=cflat, start=True, stop=True),
         reads=[b_on, b_cmpb], writes=[b_ps[2]])
    p.op(V, lambda e: e.tensor_copy(out=sc1[:].rearrange("p i j -> p (i j)"), in_=ps[2][:, 0:4 * NJ]), reads=[b_ps[2]], writes=[b_sc1])
    src, b_src, dst, b_dst = sc1, b_sc1, sc2, b_sc2
    s = 1
    while s < NJ:
        p.op(V, lambda e, s=s, src=src, dst=dst: e.tensor_tensor(out=dst[:, :, s:], in0=src[:, :, s:], in1=src[:, :, 0:NJ - s], op=ALU.add),
             reads=[b_src], writes=[b_dst])
        p.op(V, lambda e, s=s, src=src, dst=dst: e.tensor_copy(out=dst[:, :, 0:s], in_=src[:, :, 0:s]), reads=[b_src], writes=[b_dst])
        src, b_src, dst, b_dst = dst, b_dst, src, b_src
        s *= 2
    incl, b_incl = src, b_src
    p.op(V, lambda e: e.tensor_tensor(out=slotc[:].rearrange("p i j -> p (i j)"), in0=incl[:].rearrange("p i j -> p (i j)"),
                                      in1=ps[2][:, 0:4 * NJ], op=ALU.subtract), reads=[b_incl, b_ps[2]], writes=[b_slot])
    p.op(V, lambda e: e.tensor_tensor(out=slotc[:].rearrange("p i j -> p (i j)"), in0=slotc[:].rearrange("p i j -> p (i j)"),
                                      in1=ps[1][:, 0:4 * NJ], op=ALU.add), reads=[b_slot, b_ps[1]], writes=[b_slot])
    p.op(V, lambda e: e.tensor_tensor(out=slotc[:], in0=slotc[:], in1=cmp_[:], op=ALU.mult), reads=[b_slot, b_cmp], writes=[b_slot])
    p.op(V, lambda e: e.tensor_scalar(out=slotc[:], in0=slotc[:], scalar1=-1.0, scalar2=None, op0=ALU.add), reads=[b_slot], writes=[b_slot])
    p.dma("sync", slot_out, slotc[:].rearrange("p i j -> p (i j)"), "slo", reads=[b_slot])

    xinT = p.sb("xinT", [128, 16, CAP], BF16); b_xin = Buf("xin")
    hidT = p.sb("hidT", [128, 8, CAP], BF16); b_hid = Buf("hid")
    wdt = p.sb("wdt", [128, 8, D], BF16); b_wd = Buf("wd")
    wgt = [p.sb("wgt%d" % i, [128, 16, 128], BF16) for i in range(2)]; b_wg = [Buf("wg0"), Buf("wg1")]
    wut = [p.sb("wut%d" % i, [128, 16, 128], BF16) for i in range(2)]; b_wu = [Buf("wu0"), Buf("wu1")]
    hch = [p.sb("hch%d" % i, [128, (8 // (CAP // 512)) * 128], BF16) for i in range(3)]; b_hch = [Buf("h0"), Buf("h1"), Buf("h2")]
    sel = [p.sb("sel%d" % i, [128, CAP], BF16) for i in range(2)]; b_sel = [Buf("sel0"), Buf("sel1")]
    act = [p.sb("act%d" % i, [128, 512], F32) for i in range(2)]; b_act = [Buf("a0"), Buf("a1")]
    yb = [p.sb("yb%d" % i, [128, D], BF16) for i in range(2)]; b_yb = [Buf("y0"), Buf("y1")]
    n = {"h": 0, "s": 0, "ev": 0, "w": 0, "a": 0, "y": 0}
    NDG = 8 // NSH
    for i in range(4):
        b, el = i // 2, i % 2
        for pas in range(16 // NDG):
            for j in range(NJ):
                hs = n["h"] % 3; n["h"] += 1
                ss = n["s"] % 2; n["s"] += 1
                p.dma("sync", hch[hs][:, 0:NDG * 128], h_tm[b * S + j * 128:b * S + (j + 1) * 128, pas * NDG * 128:(pas + 1) * NDG * 128],
                      "hch%d" % hs, writes=[b_hch[hs]])
                p.op(V, lambda e, ss=ss, i=i, j=j: e.tensor_scalar(out=sel[ss][:], in0=iota, scalar1=slotc[:, i, j:j + 1], scalar2=None,
                                                                  op0=ALU.is_equal), reads=[b_cs, b_slot], writes=[b_sel[ss]])
                for dl in range(NDG):
                    for hf in range(NSH):
                        bk = dl * NSH + hf
                        p.op("tensor", lambda e, hs=hs, ss=ss, dl=dl, hf=hf, bk=bk, j=j: e.matmul(
                            ps[bk][:], lhsT=hch[hs][:, dl * 128:(dl + 1) * 128], rhs=sel[ss][:, hf * 512:(hf + 1) * 512],
                            start=(j == 0), stop=(j == NJ - 1)), reads=[b_hch[hs], b_sel[ss]], writes=[b_ps[bk]],
                            count=(j == NJ - 1 or (dl == NDG - 1 and hf == NSH - 1)))
            for dl in range(NDG):
                for hf in range(NSH):
                    bk = dl * NSH + hf
                    eng = "scalar" if n["ev"] % 2 == 0 else V
                    n["ev"] += 1
                    dstap = xinT[:, pas * NDG + dl, hf * 512:(hf + 1) * 512]
                    if eng == "scalar":
                        p.op(eng, lambda e, bk=bk, dstap=dstap: e.copy(out=dstap, in_=ps[bk][:]), reads=[b_ps[bk]], writes=[b_xin])
                    else:
                        p.op(eng, lambda e, bk=bk, dstap=dstap: e.tensor_copy(out=dstap, in_=ps[bk][:]), reads=[b_ps[bk]], writes=[b_xin])
        p.dma("gpsimd", wdt[:], wd[el].rearrange("(k p) n -> p k n", p=128), "wd", writes=[b_wd])
        wg_v = wg[el].rearrange("(k p) f -> p k f", p=128)
        wu_v = wu[el].rearrange("(k p) f -> p k f", p=128)
        for fc in range(8):
            ws = n["w"] % 2; n["w"] += 1
            p.dma("gpsimd", wgt[ws][:], wg_v[:, :, fc * 128:(fc + 1) * 128], "wg%d" % ws, writes=[b_wg[ws]])
            p.dma("gpsimd", wut[ws][:], wu_v[:, :, fc * 128:(fc + 1) * 128], "wu%d" % ws, writes=[b_wu[ws]])
            for hf in range(NSH):
                bg = (n["a"] % 2) * 2; n["a"] += 1
                for k in range(16):
                    p.op("tensor", lambda e, ws=ws, k=k, hf=hf, bg=bg: e.matmul(ps[bg][:], lhsT=wgt[ws][:, k, :],
                                                                               rhs=xinT[:, k, hf * 512:(hf + 1) * 512],
                                                                               start=(k == 0), stop=(k == 15)),
                         reads=[b_wg[ws], b_xin], writes=[b_ps[bg]], count=(k == 15))
                for k in range(16):
                    p.op("tensor", lambda e, ws=ws, k=k, hf=hf, bg=bg: e.matmul(ps[bg + 1][:], lhsT=wut[ws][:, k, :],
                                                                               rhs=xinT[:, k, hf * 512:(hf + 1) * 512],
                                                                               start=(k == 0), stop=(k == 15)),
                         reads=[b_wu[ws], b_xin], writes=[b_ps[bg + 1]], count=(k == 15))
                a = bg // 2
                p.op("scalar", lambda e, bg=bg, a=a: e.activation(out=act[a][:], in_=ps[bg][:], func=AF.Silu),
                     reads=[b_ps[bg]], writes=[b_act[a]])
                p.op(V, lambda e, bg=bg, a=a, fc=fc, hf=hf: e.tensor_tensor(out=hidT[:, fc, hf * 512:(hf + 1) * 512], in0=act[a][:],
                                                                          in1=ps[bg + 1][:], op=ALU.mult),
                     reads=[b_act[a], b_ps[bg + 1]], writes=[b_hid])
        for sc in range(CAP // 128):
            ys = n["y"] % 2; n["y"] += 1
            for dq in range(4):
                bk = 4 + dq
                for fc in range(8):
                    p.op("tensor", lambda e, sc=sc, dq=dq, fc=fc, bk=bk: e.matmul(ps[bk][:], lhsT=hidT[:, fc, sc * 128:(sc + 1) * 128],
                                                                                 rhs=wdt[:, fc, dq * 512:(dq + 1) * 512],
                                                                                 start=(fc == 0), stop=(fc == 7)),
                         reads=[b_hid, b_wd], writes=[b_ps[bk]], count=(fc == 7))
                eng = "scalar" if dq % 2 == 0 else V
                if eng == "scalar":
                    p.op(eng, lambda e, ys=ys, dq=dq, bk=bk: e.copy(out=yb[ys][:, dq * 512:(dq + 1) * 512], in_=ps[bk][:]),
                         reads=[b_ps[bk]], writes=[b_yb[ys]])
                else:
                    p.op(eng, lambda e, ys=ys, dq=dq, bk=bk: e.tensor_copy(out=yb[ys][:, dq * 512:(dq + 1) * 512], in_=ps[bk][:]),
                         reads=[b_ps[bk]], writes=[b_yb[ys]])
            p.dma("sync", y_out[i, sc * 128:(sc + 1) * 128, :], yb[ys][:], "yo%d" % ys, reads=[b_yb[ys]])
    return p.finish()


def k4_consts(CAP=1024):
    UT = np.triu(np.ones((128, 128), np.float32))
    io = np.broadcast_to(np.arange(CAP, dtype=np.float32)[None, :], (128, CAP))
    return np.ascontiguousarray(np.concatenate([UT, io], axis=1))


def k4_inputs(c, aff, h_tm, z, L, S=8192):
    NJ = S // 128
    affp = np.zeros((128, 4, NJ), np.float32)
    for i in range(4):
        b, el = i // 2, i % 2
        affp[:, i, :] = aff[b * S:(b + 1) * S, 2 * c + el].reshape(NJ, 128).T
    return {"affp": affp.reshape(128, 4 * NJ), "h_tm": h_tm, "wg": z["w_gate"][L][2 * c:2 * c + 2], "wu": z["w_up"][L][2 * c:2 * c + 2],
            "wd": z["w_down"][L][2 * c:2 * c + 2], "cst": k4_consts()}


def build_k5(S=8192, CAP=1024, NE=16):
    p = Prog()
    NSC = CAP // 128
    NTT = S // 512
    y_all = p.dram("y_all", [2 * NE, CAP, 256], BF16, "ExternalInput")
    slot_all = p.dram("slot_all", [2 * NE, S], F32, "ExternalInput")
    gate_all = p.dram("gate_all", [2 * NE, S], F32, "ExternalInput")
    x1s = p.dram("x1s", [256, 2 * S], F32, "ExternalInput")
    iotac = p.dram("iotac", [128, NSC], F32, "ExternalInput")
    x2s = p.dram("x2s", [256, 2 * S], F32, "ExternalOutput")

    yt = p.sb("yt", [128, NE, NSC, 256], BF16); b_y = Buf("y")
    io = p.sb("io", [128, NSC], F32); b_io = Buf("io")
    sB = [p.sb("sB%d" % i, [128, 512], F32) for i in range(2)]; b_sB = [Buf("sB0"), Buf("sB1")]
    gB = [p.sb("gB%d" % i, [128, 512], F32) for i in range(2)]; b_gB = [Buf("gB0"), Buf("gB1")]
    selT = [p.sb("selT%d" % i, [128, 512], BF16) for i in range(4)]; b_sel = [Buf("sel%d" % i) for i in range(4)]
    tmp = [p.sb("tmp%d" % i, [128, 512], F32) for i in range(2)]; b_tmp = [Buf("tmp0"), Buf("tmp1")]
    xt = [p.sb("xt%d" % i, [128, 2, 512], F32) for i in range(2)]; b_xt = [Buf("xt0"), Buf("xt1")]
    ps = [p.ps("ps%d" % i, [128, 512]) for i in range(4)]; b_ps = [Buf("ps%d" % i) for i in range(4)]

    p.dma("sync", io[:], iotac, "io", writes=[b_io])
    x1_v = x1s.rearrange("(k p) t -> p k t", p=128)
    x2_v = x2s.rearrange("(k p) t -> p k t", p=128)
    n = {"b": 0, "s": 0, "t": 0, "x": 0, "p": 0}
    for b in range(2):
        for e in range(NE):
            p.dma("sync", yt[:, e, :, :], y_all[b * NE + e].rearrange("(sc p) d -> p sc d", p=128), "y", writes=[b_y])
        for tt in range(NTT):
            t0 = tt * 512
            xs = n["x"] % 2; n["x"] += 1
            pb = (n["p"] % 2) * 2; n["p"] += 1
            p.dma("sync", xt[xs][:], x1_v[:, :, b * S + t0:b * S + t0 + 512], "xt%d" % xs, writes=[b_xt[xs]])
            for e in range(NE):
                bs = n["b"] % 2; n["b"] += 1
                r = b * NE + e
                p.dma("sync", sB[bs][:], slot_all[r, t0:t0 + 512].partition_broadcast(128), "sB%d" % bs, writes=[b_sB[bs]])
                p.dma("sync", gB[bs][:], gate_all[r, t0:t0 + 512].partition_broadcast(128), "gB%d" % bs, writes=[b_gB[bs]])
                for sc in range(NSC):
                    ss = n["s"] % 4; n["s"] += 1
                    if sc % 2 == 0:
                        p.op("vector", lambda e_, bs=bs, ss=ss, sc=sc: e_.scalar_tensor_tensor(
                            out=selT[ss][:], in0=sB[bs][:], scalar=io[:, sc:sc + 1], in1=gB[bs][:], op0=ALU.is_equal, op1=ALU.mult),
                            reads=[b_sB[bs], b_gB[bs], b_io], writes=[b_sel[ss]])
                    else:
                        ts_ = n["t"] % 2; n["t"] += 1
                        p.op("gpsimd", lambda e_, bs=bs, ts_=ts_, sc=sc: e_.tensor_scalar(
                            out=tmp[ts_][:], in0=sB[bs][:], scalar1=io[:, sc:sc + 1], scalar2=None, op0=ALU.is_equal),
                            reads=[b_sB[bs], b_io], writes=[b_tmp[ts_]])
                        p.op("gpsimd", lambda e_, bs=bs, ts_=ts_, ss=ss: e_.tensor_tensor(
                            out=selT[ss][:], in0=tmp[ts_][:], in1=gB[bs][:], op=ALU.mult),
                            reads=[b_tmp[ts_], b_gB[bs]], writes=[b_sel[ss]])
                    first = (e == 0 and sc == 0)
                    last = (e == NE - 1 and sc == NSC - 1)
                    for dc in range(2):
                        p.op("tensor", lambda e_, e=e, sc=sc, dc=dc, ss=ss, pb=pb, first=first, last=last: e_.matmul(
                            ps[pb + dc][:], lhsT=yt[:, e, sc, dc * 128:(dc + 1) * 128], rhs=selT[ss][:], start=first, stop=last),
                            reads=[b_y, b_sel[ss]], writes=[b_ps[pb + dc]], count=(dc == 1 or last))
            for dc in range(2):
                p.op("vector", lambda e_, xs=xs, dc=dc, pb=pb: e_.tensor_tensor(out=xt[xs][:, dc, :], in0=xt[xs][:, dc, :],
                                                                             in1=ps[pb + dc][:], op=ALU.add),
                     reads=[b_xt[xs], b_ps[pb + dc]], writes=[b_xt[xs]])
            p.dma("sync", x2_v[:, :, b * S + t0:b * S + t0 + 512], xt[xs][:], "xo%d" % xs, reads=[b_xt[xs]])
    return p.finish()


_PROGS = {}


def _prog(name, fn):
    if name not in _PROGS:
        _PROGS[name] = fn()
    return _PROGS[name]


def kernel(**inp):
    z = {k: np.asarray(v) for k, v in inp.items()}
    NT = 16384
    x_tm = np.ascontiguousarray(z["x"].reshape(NT, 2048).astype(np.float32))
    for L in range(4):
        nw = np.ascontiguousarray(z["mix_norm_w"][L].reshape(16, 128).T)
        ins = [{"xT": np.ascontiguousarray(x_tm[c * 2048:(c + 1) * 2048].T), "nw": nw, "w_in": z["w_in"][L]} for c in range(8)]
        r = run(_prog("k1", build_k1), ins).results
        ufm = np.concatenate([r[c]["ufm"] for c in range(8)], axis=1)
        utm = np.concatenate([r[c]["utm"] for c in range(8)], axis=0)
        udt = np.concatenate([r[c]["udt"] for c in range(8)], axis=0)
        del r, ins
        ins = [k2a_inputs(c // 4, c % 4, ufm, utm, z, L) for c in range(8)]
        r = run(_prog("k2a", build_k2a), ins).results
        o_na = np.zeros((NT, 512), NPBF)
        o_wa = np.zeros((NT, 512), NPBF)
        for c in range(8):
            b, j = c // 4, c % 4
            o_na[b * 8192:(b + 1) * 8192, j * 128:(j + 1) * 128] = r[c]["o_na"]
            o_wa[b * 8192:(b + 1) * 8192, j * 128:(j + 1) * 128] = r[c]["o_wa"]
        del r, ins
        ins = [k2c_inputs(c // 4, c % 4, ufm, udt, z, L) for c in range(8)]
        r = run(_prog("k2c", build_k2c), ins).results
        y = np.zeros((NT, 512), np.float32)
        for c in range(8):
            b, j = c // 4, c % 4
            y[b * 8192:(b + 1) * 8192, j * 128:(j + 1) * 128] = r[c]["y_out"]
        del r, ins
        ins = [k3_inputs(c, x_tm, o_na, o_wa, y, ufm, z, L) for c in range(8)]
        r = run(_prog("k3", build_k3), ins).results
        x1_tm = np.concatenate([r[c]["x1T"].T for c in range(8)], axis=0)
        h_tm = np.ascontiguousarray(np.concatenate([r[c]["hT"].T for c in range(8)], axis=0))
        aff = np.concatenate([r[c]["aff"] for c in range(8)], axis=0)
        del r, ins, ufm, utm, udt
        ins = [k4_inputs(c, aff, h_tm, z, L) for c in range(8)]
        r = run(_prog("k4", build_k4), ins).results
        Y = [r[c]["y_out"] for c in range(8)]
        SL = [r[c]["slot_out"].reshape(128, 4, 64) for c in range(8)]
        del r, ins, h_tm
        slot_all = np.zeros((32, 8192), np.float32)
        gate_all = np.zeros((32, 8192), np.float32)
        for b in range(2):
            for e in range(16):
                slot_all[b * 16 + e] = SL[e // 2][:, b * 2 + e % 2, :].T.reshape(8192)
                gate_all[b * 16 + e] = aff[b * 8192:(b + 1) * 8192, e]
        iotac = (np.arange(8)[None, :] * 128 + np.arange(128)[:, None]).astype(np.float32)
        ins = []
        for c in range(8):
            y_all = np.zeros((32, 1024, 256), NPBF)
            for b in range(2):
                for e in range(16):
                    y_all[b * 16 + e] = Y[e // 2][b * 2 + e % 2][:, 256 * c:256 * c + 256]
            ins.append({"y_all": y_all, "slot_all": slot_all, "gate_all": gate_all,
                        "x1s": np.ascontiguousarray(x1_tm[:, 256 * c:256 * c + 256].T), "iotac": iotac})
        r = run(_prog("k5", build_k5), ins).results
        x_tm = np.empty((NT, 2048), np.float32)
        for c in range(8):
            x_tm[:, 256 * c:256 * c + 256] = r[c]["x2s"].T
        del r, ins, Y, SL
        print("layer", L, "done; x std", float(x_tm.std()), "nan", int(np.isnan(x_tm).sum()), flush=True)
    return x_tm.reshape(2, 8192, 2048).astype(np.float32)
```

```python
import numpy as np
import ml_dtypes
from contextlib import ExitStack
import concourse.bass as bass
import concourse.mybir as mybir
from concourse.bass_utils import run_bass_kernel_spmd

F32 = mybir.dt.float32
BF16 = mybir.dt.bfloat16
I32 = mybir.dt.int32
AF = mybir.ActivationFunctionType
ALU = mybir.AluOpType
AX = mybir.AxisListType
NPBF = ml_dtypes.bfloat16

ENG = ("sync", "gpsimd", "tensor", "vector", "scalar")


class Buf:
    __slots__ = ("name", "w", "r")

    def __init__(self, name):
        self.name = name
        self.w = None
        self.r = []


class Prog:
    def __init__(self):
        self.nc = bass.Bass("TRN2", target_bir_lowering=False)
        self.es = ExitStack()
        self.q = {e: [] for e in ENG}
        self.cnt = {}
        self.semh = {}
        self.pe_pending = []

    def dram(self, name, shape, dt, kind="Internal"):
        return self.nc.dram_tensor(name, list(shape), dt, kind=kind).ap()

    def sb(self, name, shape, dt):
        return self.es.enter_context(self.nc.sbuf_tensor(name, list(shape), dt))

    def ps(self, name, shape, dt=F32):
        return self.es.enter_context(self.nc.psum_tensor(name, list(shape), dt))

    def _sem(self, key):
        if key not in self.semh:
            nm = "s" + str(len(self.semh))
            self.semh[key] = self.es.enter_context(self.nc.semaphore(nm))
            self.cnt[key] = 0
        return self.semh[key]

    def _deps(self, eng, reads, writes):
        waits = []
        for b in reads:
            if b.w is not None:
                waits.append(b.w)
        for b in writes:
            if b.w is not None:
                waits.append(b.w)
            for t in b.r:
                waits.append(t)
        return waits

    def op(self, eng, fn, reads=(), writes=(), count=True):
        waits = self._deps(eng, reads, writes)
        if eng == "tensor":
            waits = [w for w in waits if w[0] != "tensor"]
        if count:
            self._sem(eng)
            self.cnt[eng] += 1
            tok = (eng, self.cnt[eng])
        else:
            tok = None
        self.q[eng].append((fn, waits, eng if count else None, 1))
        if eng == "tensor":
            if tok is None:
                self.pe_pending.extend(reads)
                return None
            for b in self.pe_pending:
                b.r.append(tok)
            self.pe_pending = []
        for b in reads:
            b.r.append(tok)
        for b in writes:
            b.w = tok
            b.r = []
        return tok

    def dma(self, eng, out, in_, key, reads=(), writes=(), **kw):
        waits = self._deps(eng, reads, writes)
        k = ("d", key)
        self._sem(k)
        self.cnt[k] += 16
        tok = (k, self.cnt[k])
        self.q[eng].append((lambda e: e.dma_start(out=out, in_=in_, **kw), waits, k, 16))
        for b in reads:
            b.r.append(tok)
        for b in writes:
            b.w = tok
            b.r = []
        return tok

    def raw(self, eng, fn, key, inc, reads=(), writes=()):
        waits = self._deps(eng, reads, writes)
        k = ("d", key)
        self._sem(k)
        self.cnt[k] += inc
        tok = (k, self.cnt[k])
        self.q[eng].append((fn, waits, k, inc))
        for b in reads:
            b.r.append(tok)
        for b in writes:
            b.w = tok
            b.r = []
        return tok

    def breg(self, e, val):
        d = self.__dict__.setdefault("_bregs", {})
        if val not in d:
            d[val] = e.to_reg(val)
        return d[val]

    def finish(self):
        finals = [(k, v) for k, v in self.cnt.items() if isinstance(k, tuple)]
        semh = self.semh
        with self.nc.Block() as block:
            for eng in ENG:
                items = self.q[eng]
                if not items and eng != "sync":
                    continue

                def body(e, items=items, eng=eng):
                    waited = {}
                    for fn, waits, key, inc in items:
                        for (k, v) in waits:
                            if waited.get(k, 0) >= v:
                                continue
                            e.wait_ge(semh[k], v)
                            waited[k] = v
                        ins = fn(e)
                        if key is not None:
                            ins.then_inc(semh[key], inc)
                    if eng == "sync":
                        for k, v in finals:
                            e.wait_ge(semh[k], v)
                getattr(block, eng)(body)
        self.es.close()
        return self.nc


def run(nc, in_maps, trace=False):
    res = run_bass_kernel_spmd(nc, in_maps, core_ids=list(range(len(in_maps))), trace=trace)
    return res


D = 2048
INW = 4880
KC = D // 128
EPS = 1e-6
FM_RANGES = [(0, 1024), (1536, 640), (2304, 1536), (3856, 1024)]
NFM = sum(w for _, w in FM_RANGES) // 128
TM_RANGES = [(1024, 512), (2176, 128)]
DT_RANGE = (3840, 16)


def build_k1(T=2048):
    p = Prog()
    nc = p.nc
    xT = p.dram("xT", [D, T], F32, "ExternalInput")
    nw = p.dram("nw", [128, KC], F32, "ExternalInput")
    w_in = p.dram("w_in", [D, INW], F32, "ExternalInput")
    ufm = p.dram("ufm", [NFM * 128, T], BF16, "ExternalOutput")
    utm = p.dram("utm", [T, 640], BF16, "ExternalOutput")
    udt = p.dram("udt", [T, 16], F32, "ExternalOutput")

    TT = 512
    NT = T // TT
    hnT = p.sb("hnT", [128, KC, T], BF16)
    b_hn = [Buf("hn%d" % i) for i in range(NT)]
    nwt = p.sb("nwt", [128, KC], F32)
    b_nw = Buf("nw")
    ones = p.sb("ones", [128, 128], BF16)
    b_ones = Buf("ones")
    xt = [p.sb("xt%d" % i, [128, KC, TT], F32) for i in range(2)]
    b_xt = [Buf("xt%d" % i) for i in range(2)]
    sq = p.sb("sq", [128, KC, TT], BF16)
    b_sq = Buf("sq")
    rstd = p.sb("rstd", [128, TT], F32)
    b_rstd = Buf("rstd")
    ps_ss = p.ps("ps_ss", [128, TT])
    b_pss = Buf("pss")
    pss = [p.ps("psm%d" % i, [128, 512]) for i in range(4)]
    b_ps = [Buf("psm%d" % i) for i in range(4)]
    wp = [p.sb("wp%d" % i, [128, KC, 512], BF16) for i in range(2)]
    b_wp = [Buf("wp%d" % i) for i in range(2)]
    ost = [p.sb("ost%d" % i, [128, T], BF16) for i in range(2)]
    b_ost = [Buf("ost%d" % i) for i in range(2)]
    otm = [p.sb("otm%d" % i, [128, 512], BF16) for i in range(2)]
    b_otm = [Buf("otm%d" % i) for i in range(2)]
    odt = [p.sb("odt%d" % i, [128, 16], F32) for i in range(2)]
    b_odt = [Buf("odt%d" % i) for i in range(2)]

    epst = p.sb("epst", [128, 1], F32)
    b_eps = Buf("eps")
    p.op("gpsimd", lambda e: e.memset(epst[:], EPS), writes=[b_eps])
    p.dma("sync", nwt[:], nw, "nw", writes=[b_nw])
    p.op("gpsimd", lambda e: e.memset(ones[:], 1.0), writes=[b_ones])

    xT_v = xT.rearrange("(k p) t -> p k t", p=128)
    w_v = w_in.rearrange("(k p) n -> p k n", p=128)

    for tt in range(NT):
        s = tt % 2
        p.dma("sync", xt[s][:], xT_v[:, :, tt * TT:(tt + 1) * TT], "xt%d" % s, writes=[b_xt[s]])
        p.op("scalar", lambda e, s=s: e.activation(out=sq[:], in_=xt[s][:], func=AF.Square),
             reads=[b_xt[s]], writes=[b_sq])
        for k in range(KC):
            p.op("tensor", lambda e, k=k: e.matmul(ps_ss[:], lhsT=ones[:], rhs=sq[:, k, :],
                                                    start=(k == 0), stop=(k == KC - 1)),
                 reads=[b_ones, b_sq], writes=[b_pss], count=(k == KC - 1))
        p.op("scalar", lambda e: e.activation(out=rstd[:], in_=ps_ss[:], func=AF.Sqrt, bias=epst[:], scale=1.0 / D),
             reads=[b_pss, b_eps], writes=[b_rstd])
        p.op("vector", lambda e: e.reciprocal(out=rstd[:], in_=rstd[:]),
             reads=[b_rstd], writes=[b_rstd])
        for k in range(KC):
            eng = "vector"
            p.op(eng, lambda e, k=k, s=s, tt=tt: e.scalar_tensor_tensor(
                out=hnT[:, k, tt * TT:(tt + 1) * TT], in0=xt[s][:, k, :], scalar=nwt[:, k:k + 1],
                in1=rstd[:], op0=ALU.mult, op1=ALU.mult),
                reads=[b_xt[s], b_rstd, b_nw], writes=[b_hn[tt]])

    panels = []
    row = 0
    for c0, w in FM_RANGES:
        o = 0
        while o < w:
            pw = min(512, w - o)
            panels.append((c0 + o, pw, "fm", row))
            row += pw // 128
            o += pw
    panels.append((TM_RANGES[0][0], 512, "tm", 0))
    panels.append((TM_RANGES[1][0], 128, "tm", 512))
    panels.append((DT_RANGE[0], 16, "dt", 0))

    ev = 0
    psi = 0
    osi = 0
    for pi, (c0, pw, kind, o0) in enumerate(panels):
        s = pi % 2
        p.dma("gpsimd", wp[s][:, :, 0:pw], w_v[:, :, c0:c0 + pw], "wp%d" % s, writes=[b_wp[s]])
        if kind == "fm":
            for ci in range(pw // 128):
                so = osi % 2
                osi += 1
                for tt in range(NT):
                    b = psi % 4
                    psi += 1
                    for k in range(KC):
                        p.op("tensor", lambda e, b=b, s=s, ci=ci, k=k, tt=tt: e.matmul(
                            pss[b][:], lhsT=wp[s][:, k, ci * 128:(ci + 1) * 128],
                            rhs=hnT[:, k, tt * TT:(tt + 1) * TT], start=(k == 0), stop=(k == KC - 1)),
                            reads=[b_wp[s], b_hn[tt]], writes=[b_ps[b]], count=(k == KC - 1))
                    eng = "scalar" if ev % 2 == 0 else "vector"
                    ev += 1
                    if eng == "scalar":
                        p.op(eng, lambda e, b=b, so=so, tt=tt: e.copy(out=ost[so][:, tt * TT:(tt + 1) * TT], in_=pss[b][:]),
                             reads=[b_ps[b]], writes=[b_ost[so]])
                    else:
                        p.op(eng, lambda e, b=b, so=so, tt=tt: e.tensor_copy(out=ost[so][:, tt * TT:(tt + 1) * TT], in_=pss[b][:]),
                             reads=[b_ps[b]], writes=[b_ost[so]])
                r0 = (o0 + ci) * 128
                p.dma("sync", ufm[r0:r0 + 128, :], ost[so][:], "ost%d" % so, reads=[b_ost[so]])
        else:
            for t8 in range(T // 128):
                b = psi % 4
                psi += 1
                for k in range(KC):
                    p.op("tensor", lambda e, b=b, s=s, k=k, t8=t8, pw=pw: e.matmul(
                        pss[b][:, 0:pw], lhsT=hnT[:, k, t8 * 128:(t8 + 1) * 128],
                        rhs=wp[s][:, k, 0:pw], start=(k == 0), stop=(k == KC - 1)),
                        reads=[b_wp[s], b_hn[t8 // 4]], writes=[b_ps[b]], count=(k == KC - 1))
                so = t8 % 2
                if kind == "tm":
                    p.op("vector", lambda e, b=b, so=so, pw=pw: e.tensor_copy(out=otm[so][:, 0:pw], in_=pss[b][:, 0:pw]),
                         reads=[b_ps[b]], writes=[b_otm[so]])
                    p.dma("sync", utm[t8 * 128:(t8 + 1) * 128, o0:o0 + pw], otm[so][:, 0:pw], "otm%d" % so,
                          reads=[b_otm[so]])
                else:
                    p.op("vector", lambda e, b=b, so=so, pw=pw: e.tensor_copy(out=odt[so][:, 0:pw], in_=pss[b][:, 0:pw]),
                         reads=[b_ps[b]], writes=[b_odt[so]])
                    p.dma("sync", udt[t8 * 128:(t8 + 1) * 128, :], odt[so][:], "odt%d" % so, reads=[b_odt[so]])
    return p.finish()


S = 8192
NQT = S // 128
EPS = 1e-6
NEG = -30000.0


def na_tiles():
    out = []
    for qp in range(NQT):
        if qp == 0:
            out.append(([0, 1, 2, 3], [5, 6, 7, 8]))
        elif qp == 1:
            out.append(([0, 1, 2, 3], [9, 10, 11, 12]))
        elif qp == 62:
            out.append(([60, 61, 62, 63], [13, 14, 15, 16]))
        elif qp == 63:
            out.append(([60, 61, 62, 63], [17, 18, 19, 20]))
        else:
            out.append(([qp - 2, qp - 1, qp, qp + 1, qp + 2], [0, 1, 2, 3, 4]))
    return out


def na_bias_host(rpb):
    H = rpb.shape[0]
    tl = na_tiles()
    cases = [(2, 0)] * 0
    res = np.full((H, 21, 128, 128), NEG, np.float32)
    done = set()
    for qp, (kps, tis) in enumerate(tl):
        for kp, ti in zip(kps, tis):
            if ti in done:
                continue
            done.add(ti)
            qi = np.arange(128)
            r = 2 * qp + qi // 64
            c = qi % 64
            r0 = np.clip(r - 4, 0, 120)
            c0 = np.clip(c - 8, 0, 48)
            ki = np.arange(128)
            kr = 2 * kp + ki // 64
            kc = ki % 64
            inwin = ((kr[:, None] >= r0[None, :]) & (kr[:, None] < r0[None, :] + 8) &
                     (kc[:, None] >= c0[None, :]) & (kc[:, None] < c0[None, :] + 16))
            di = np.clip(kr[:, None] - r[None, :] + 7, 0, 14)
            dj = np.clip(kc[:, None] - c[None, :] + 15, 0, 30)
            vals = rpb[:, di, dj]
            res[:, ti] = np.where(inwin[None], vals, np.float32(NEG))
    return res


def wa_mask_host():
    ki = np.arange(128)[:, None]
    qi = np.arange(128)[None, :]
    m = np.zeros((3, 128, 128), np.float32)
    m[0] = np.where(ki >= qi, 0.0, NEG)
    m[2] = np.where(ki <= qi, 0.0, NEG)
    return m


def rope_tables_host():
    half = 8
    inv = 1.0 / (500000.0 ** (np.arange(half, dtype=np.float32) * 2.0 / 16))
    ang = np.arange(S, dtype=np.float32)[None, :] * inv[:, None].astype(np.float32)
    cos = np.cos(ang).astype(np.float32)
    sin = np.sin(ang).astype(np.float32)
    cT = np.ones((64, S), np.float32)
    sT = np.zeros((64, S), np.float32)
    cT[0:8] = cos
    cT[8:16] = cos
    sT[0:8] = -sin
    sT[8:16] = sin
    pm = np.zeros((64, 64), np.float32)
    for i in range(8):
        pm[i, i + 8] = 1.0
        pm[i + 8, i] = 1.0
    return cT, sT, pm


def build_k2a():
    p = Prog()
    na_qT = p.dram("na_qT", [128, S], BF16, "ExternalInput")
    na_kT = p.dram("na_kT", [128, S], BF16, "ExternalInput")
    na_v = p.dram("na_v", [S, 128], BF16, "ExternalInput")
    wa_qT = p.dram("wa_qT", [128, S], BF16, "ExternalInput")
    wa_kT = p.dram("wa_kT", [64, S], BF16, "ExternalInput")
    wa_v = p.dram("wa_v", [S, 64], BF16, "ExternalInput")
    nrm = p.dram("nrm", [128, 4], F32, "ExternalInput")
    na_bias = p.dram("na_bias", [128, 2 * 21 * 128], F32, "ExternalInput")
    wa_mask = p.dram("wa_mask", [128, 3 * 128], F32, "ExternalInput")
    cst = p.dram("cst", [128, 3 * 128], F32, "ExternalInput")
    ropeT = p.dram("ropeT", [128, 2 * S], F32, "ExternalInput")
    sink = p.dram("sink", [128, 2], F32, "ExternalInput")
    o_na = p.dram("o_na", [S, 128], BF16, "ExternalOutput")
    o_wa = p.dram("o_wa", [S, 128], BF16, "ExternalOutput")

    qn = p.sb("qn", [128, S], BF16); b_qn = Buf("qn")
    kn = p.sb("kn", [128, S], BF16); b_kn = Buf("kn")
    raw = [p.sb("raw%d" % i, [128, S], BF16) for i in range(2)]; b_raw = [Buf("raw0"), Buf("raw1")]
    vaug = p.sb("vaug", [128, NQT, 2, 65], BF16); b_va = Buf("vaug")
    vraw = p.sb("vraw", [128, NQT, 128], BF16); b_vr = Buf("vraw")
    nrt = p.sb("nrt", [128, 4], F32); b_nr = Buf("nrt")
    nr8 = p.sb("nr8", [128, 4], F32); b_nr8 = Buf("nr8")
    bias = p.sb("bias", [128, 2 * 21 * 128], BF16); b_bias = Buf("bias")
    wmask = p.sb("wmask", [128, 3 * 128], BF16); b_wm = Buf("wmask")
    cs = p.sb("cs", [128, 3 * 128], BF16); b_cs = Buf("cs")
    ident = cs[:, 0:128]
    bones = cs[:, 128:256]
    pmat = cs[:, 256:384]
    skt = p.sb("skt", [128, 2], F32); b_sk = Buf("skt")
    epst = p.sb("epst", [128, 1], F32); b_eps = Buf("eps")
    sq = [p.sb("sq%d" % i, [128, 512], BF16) for i in range(2)]; b_sq = [Buf("sq0"), Buf("sq1")]
    rstd = [p.sb("rstd%d" % i, [128, 512], F32) for i in range(2)]; b_rstd = [Buf("r0"), Buf("r1")]
    tmpn = [p.sb("tmpn%d" % i, [128, 512], F32) for i in range(2)]; b_tmpn = [Buf("t0"), Buf("t1")]
    rp = [p.sb("rp%d" % i, [128, 2, 512], F32) for i in range(2)]; b_rp = [Buf("rp0"), Buf("rp1")]
    ps_n = [p.ps("ps_n%d" % i, [128, 512]) for i in range(2)]; b_psn = [Buf("psn0"), Buf("psn1")]
    ps_s = [p.ps("ps_s%d" % i, [128, 1024]) for i in range(2)]; b_pss = [Buf("pss0"), Buf("pss1")]
    ps_o = [p.ps("ps_o%d" % i, [128, 128]) for i in range(2)]; b_pso = [Buf("pso0"), Buf("pso1")]
    E = [p.sb("E%d" % i, [128, 640], BF16) for i in range(2)]; b_E = [Buf("E0"), Buf("E1")]
    den = [p.sb("den%d" % i, [128, 1], F32) for i in range(2)]; b_den = [Buf("den0"), Buf("den1")]
    ost = [p.sb("ost%d" % i, [128, 8, 128], BF16) for i in range(2)]; b_ost = [Buf("ost0"), Buf("ost1")]

    p.op("gpsimd", lambda e: e.memset(epst[:], EPS), writes=[b_eps])
    p.dma("sync", nrt[:], nrm, "nrt", writes=[b_nr])
    p.dma("sync", skt[:], sink, "skt", writes=[b_sk])
    p.dma("gpsimd", cs[:], cst, "cs", writes=[b_cs])
    p.dma("gpsimd", bias[:], na_bias, "bias", writes=[b_bias])
    p.dma("gpsimd", wmask[:], wa_mask, "wm", writes=[b_wm])
    p.op("scalar", lambda e: e.activation(out=skt[:], in_=skt[:], func=AF.Exp), reads=[b_sk], writes=[b_sk])
    p.op("vector", lambda e: e.tensor_scalar(out=nr8[:], in0=nrt[:], scalar1=0.125, scalar2=None, op0=ALU.mult),
         reads=[b_nr], writes=[b_nr8])

    cnt = {"n": 0}

    def qknorm(src, nparts, dst, b_dst, wcol, rope):
        for tt in range(S // 512):
            i = cnt["n"] % 2
            cnt["n"] += 1
            sl = slice(tt * 512, (tt + 1) * 512)
            p.op("scalar", lambda e, i=i, sl=sl: e.activation(out=sq[i][0:nparts, :], in_=src[0:nparts, sl], func=AF.Square),
                 reads=[b_src], writes=[b_sq[i]])
            p.op("tensor", lambda e, i=i: e.matmul(ps_n[i][0:nparts, :], lhsT=bones[0:nparts, 0:nparts], rhs=sq[i][0:nparts, :],
                                                   start=True, stop=True),
                 reads=[b_cs, b_sq[i]], writes=[b_psn[i]])
            p.op("scalar", lambda e, i=i: e.activation(out=rstd[i][0:nparts, :], in_=ps_n[i][0:nparts, :], func=AF.Sqrt,
                                                       bias=epst[0:nparts, :], scale=1.0 / 64),
                 reads=[b_psn[i], b_eps], writes=[b_rstd[i]])
            p.op("vector", lambda e, i=i: e.reciprocal(out=rstd[i][0:nparts, :], in_=rstd[i][0:nparts, :]),
                 reads=[b_rstd[i]], writes=[b_rstd[i]])
            if not rope:
                p.op("vector", lambda e, i=i, sl=sl: e.scalar_tensor_tensor(
                    out=dst[0:nparts, sl], in0=src[0:nparts, sl], scalar=wcol[0:nparts, :], in1=rstd[i][0:nparts, :],
                    op0=ALU.mult, op1=ALU.mult), reads=[b_src, b_rstd[i], b_nr, b_nr8], writes=[b_dst])
            else:
                p.dma("sync", rp[i][0:nparts, 0, :], ropeT[0:nparts, sl], "rp%d" % i, writes=[b_rp[i]])
                p.dma("sync", rp[i][0:nparts, 1, :], ropeT[0:nparts, S + tt * 512:S + (tt + 1) * 512], "rp%d" % i,
                      writes=[b_rp[i]])
                p.op("vector", lambda e, i=i, sl=sl: e.scalar_tensor_tensor(
                    out=sq[i][0:nparts, :], in0=src[0:nparts, sl], scalar=wcol[0:nparts, :], in1=rstd[i][0:nparts, :],
                    op0=ALU.mult, op1=ALU.mult), reads=[b_src, b_rstd[i], b_nr, b_nr8, b_psn[i]], writes=[b_sq[i]])
                p.op("tensor", lambda e, i=i: e.matmul(ps_n[i][0:nparts, :], lhsT=pmat[0:nparts, 0:nparts], rhs=sq[i][0:nparts, :],
                                                       start=True, stop=True),
                     reads=[b_cs, b_sq[i]], writes=[b_psn[i]])
                p.op("vector", lambda e, i=i: e.tensor_tensor(out=tmpn[i][0:nparts, :], in0=ps_n[i][0:nparts, :],
                                                              in1=rp[i][0:nparts, 1, :], op=ALU.mult),
                     reads=[b_psn[i], b_rp[i]], writes=[b_tmpn[i]])
                p.op("vector", lambda e, i=i: e.tensor_tensor(out=rstd[i][0:nparts, :], in0=sq[i][0:nparts, :],
                                                              in1=rp[i][0:nparts, 0, :], op=ALU.mult),
                     reads=[b_sq[i], b_rp[i]], writes=[b_rstd[i]])
                p.op("vector", lambda e, i=i, sl=sl: e.tensor_tensor(out=dst[0:nparts, sl], in0=rstd[i][0:nparts, :],
                                                                     in1=tmpn[i][0:nparts, :], op=ALU.add),
                     reads=[b_rstd[i], b_tmpn[i]], writes=[b_dst])

    def load_v(vsrc, width, nh):
        p.dma("sync", vraw[:, :, 0:width], vsrc.rearrange("(t p) c -> p t c", p=128), "vraw", writes=[b_vr])
        p.op("gpsimd", lambda e: e.memset(vaug[:, :, :, 64:65], 1.0), writes=[b_va])
        for h in range(2):
            c0 = h * 64 if nh == 2 else 0
            p.op("vector", lambda e, h=h, c0=c0: e.tensor_copy(out=vaug[:, :, h, 0:64], in_=vraw[:, :, c0:c0 + 64]),
                 reads=[b_vr], writes=[b_va])

    oc = {"n": 0, "e": 0}

    def attention(tiles_fn, kbase_fn, bias_fn, extra_den, o_dram):
        for qg in range(NQT // 8):
            so = oc["n"] % 2
            oc["n"] += 1
            for q8 in range(8):
                qp = qg * 8 + q8
                for h in range(2):
                    kps, bts = tiles_fn(qp)
                    nk = len(kps)
                    i = oc["e"] % 2
                    oc["e"] += 1
                    kb = kbase_fn(h)
                    for j, kp in enumerate(kps):
                        bt = bts[j]
                        last = bt is None
                        p.op("tensor", lambda e, i=i, j=j, kp=kp, kb=kb, h=h, qp=qp, last=last: e.matmul(
                            ps_s[i][:, j * 128:(j + 1) * 128], lhsT=kn[kb:kb + 64, kp * 128:(kp + 1) * 128],
                            rhs=qn[h * 64:(h + 1) * 64, qp * 128:(qp + 1) * 128], start=True, stop=last),
                            reads=[b_kn, b_qn], writes=[b_pss[i]], count=(last and j == nk - 1))
                        if not last:
                            p.op("tensor", lambda e, i=i, j=j, h=h, bt=bt: e.matmul(
                                ps_s[i][:, j * 128:(j + 1) * 128], lhsT=ident, rhs=bias_fn(h, bt), start=False, stop=True),
                                reads=[b_cs, b_bias, b_wm], writes=[b_pss[i]], count=(j == nk - 1))
                    p.op("scalar", lambda e, i=i, nk=nk: e.activation(out=E[i][:, 0:nk * 128], in_=ps_s[i][:, 0:nk * 128],
                                                                      func=AF.Exp),
                         reads=[b_pss[i]], writes=[b_E[i]])
                    for j, kp in enumerate(kps):
                        p.op("tensor", lambda e, i=i, j=j, kp=kp, h=h, nk=nk: e.matmul(
                            ps_o[i][:, 0:65], lhsT=E[i][:, j * 128:(j + 1) * 128], rhs=vaug[:, kp, h, :],
                            start=(j == 0), stop=(j == nk - 1)),
                            reads=[b_E[i], b_va], writes=[b_pso[i]], count=(j == nk - 1))
                    if extra_den:
                        p.op("vector", lambda e, i=i, h=h: e.tensor_scalar(out=den[i][:], in0=ps_o[i][:, 64:65],
                                                                           scalar1=skt[:, h:h + 1], scalar2=None, op0=ALU.add),
                             reads=[b_pso[i], b_sk], writes=[b_den[i]])
                        p.op("vector", lambda e, i=i: e.reciprocal(out=den[i][:], in_=den[i][:]),
                             reads=[b_den[i]], writes=[b_den[i]])
                    else:
                        p.op("vector", lambda e, i=i: e.reciprocal(out=den[i][:], in_=ps_o[i][:, 64:65]),
                             reads=[b_pso[i]], writes=[b_den[i]])
                    p.op("vector", lambda e, i=i, h=h, so=so, q8=q8: e.tensor_scalar(
                        out=ost[so][:, q8, h * 64:(h + 1) * 64], in0=ps_o[i][:, 0:64], scalar1=den[i][:], scalar2=None,
                        op0=ALU.mult), reads=[b_pso[i], b_den[i]], writes=[b_ost[so]])
            p.dma("sync", o_dram[qg * 1024:(qg + 1) * 1024, :].rearrange("(t p) c -> p t c", p=128), ost[so][:],
                  "ost%d" % so, reads=[b_ost[so]])

    b_src = Buf("src")
    p.dma("sync", raw[0][:], na_qT, "raw0", writes=[b_raw[0]])
    p.dma("sync", raw[1][:], na_kT, "raw1", writes=[b_raw[1]])
    load_v(na_v, 128, 2)
    b_src = b_raw[0]
    qknorm(raw[0], 128, qn, b_qn, nr8[:, 0:1], False)
    b_src = b_raw[1]
    qknorm(raw[1], 128, kn, b_kn, nrt[:, 1:2], False)
    tl = na_tiles()
    attention(lambda qp: tl[qp], lambda h: h * 64,
              lambda h, bt: bias[:, (h * 21 + bt) * 128:(h * 21 + bt + 1) * 128], False, o_na)

    p.dma("sync", raw[0][:], wa_qT, "raw0", writes=[b_raw[0]])
    p.dma("sync", raw[1][0:64, :], wa_kT, "raw1", writes=[b_raw[1]])
    p.dma("sync", raw[1][64:128, :], wa_kT, "raw1", writes=[b_raw[1]])
    load_v(wa_v, 64, 1)
    b_src = b_raw[0]
    qknorm(raw[0], 128, qn, b_qn, nr8[:, 2:3], True)
    b_src = b_raw[1]
    qknorm(raw[1], 128, kn, b_kn, nrt[:, 3:4], True)

    def wa_tiles(qp):
        kps, bts = [], []
        if qp > 0:
            kps.append(qp - 1); bts.append(0)
        kps.append(qp); bts.append(None)
        if qp < NQT - 1:
            kps.append(qp + 1); bts.append(2)
        return kps, bts

    attention(wa_tiles, lambda h: h * 64, lambda h, bt: wmask[:, bt * 128:(bt + 1) * 128], True, o_wa)
    return p.finish()


def k2a_inputs(b, j, ufm, utm, z, L):
    ts = slice(b * S, (b + 1) * S)
    cT, sT, pm = rope_tables_host()
    ident = np.eye(128, dtype=np.float32)
    bo = np.zeros((128, 128), np.float32); bo[0:64, 0:64] = 1; bo[64:, 64:] = 1
    pmm = np.zeros((128, 128), np.float32); pmm[0:64, 0:64] = pm; pmm[64:, 64:] = pm
    nrm = np.zeros((128, 4), np.float32)
    nrm[:, 0] = np.tile(z["na_q_norm"][L], 2); nrm[:, 1] = np.tile(z["na_k_norm"][L], 2)
    nrm[:, 2] = np.tile(z["wa_q_norm"][L], 2); nrm[:, 3] = np.tile(z["wa_k_norm"][L], 2)
    nb = na_bias_host(z["na_rpb"][L][2 * j:2 * j + 2])
    nb = np.ascontiguousarray(nb.transpose(2, 0, 1, 3)).reshape(128, 2 * 21 * 128)
    wm = np.ascontiguousarray(wa_mask_host().transpose(1, 0, 2)).reshape(128, 384)
    sink = np.broadcast_to(z["wa_sink"][L][2 * j:2 * j + 2][None, :], (128, 2)).copy()
    return {
        "na_qT": np.ascontiguousarray(ufm[(0 + j) * 128:(1 + j) * 128, ts]),
        "na_kT": np.ascontiguousarray(ufm[(4 + j) * 128:(5 + j) * 128, ts]),
        "na_v": np.ascontiguousarray(utm[ts, j * 128:(j + 1) * 128]),
        "wa_qT": np.ascontiguousarray(ufm[(8 + j) * 128:(9 + j) * 128, ts]),
        "wa_kT": np.ascontiguousarray(ufm[12 * 128 + (j // 2) * 64:12 * 128 + (j // 2 + 1) * 64, ts]),
        "wa_v": np.ascontiguousarray(utm[ts, 512 + (j // 2) * 64:512 + (j // 2 + 1) * 64]),
        "nrm": nrm, "na_bias": nb, "wa_mask": wm,
        "cst": np.concatenate([ident, bo, pmm], axis=1),
        "ropeT": np.concatenate([np.tile(cT, (2, 1)), np.tile(sT, (2, 1))], axis=1),
        "sink": sink,
    }


S = 8192
NCH = S // 128
NEG = -30000.0


def build_k2c():
    p = Prog()
    raw_in = p.dram("raw_in", [3, 128, S + 4], BF16, "ExternalInput")
    cw = p.dram("cw", [128, 15], F32, "ExternalInput")
    cb = p.dram("cb", [128, 3], F32, "ExternalInput")
    dt_in = p.dram("dt_in", [128, NCH * 4], F32, "ExternalInput")
    prm = p.dram("prm", [128, 10], F32, "ExternalInput")
    cst = p.dram("cst", [128, 6 * 128], F32, "ExternalInput")
    y_out = p.dram("y_out", [S, 128], F32, "ExternalOutput")

    cs = p.sb("cs", [128, 6 * 128], F32); b_cs = Buf("cs")
    U = cs[:, 0:128]; UT = cs[:, 128:256]; ones = cs[:, 256:384]; identf = cs[:, 384:512]
    mnf = cs[:, 512:640]; mnb = cs[:, 640:768]
    csb = p.sb("csb", [128, 6 * 128], BF16); b_csb = Buf("csb")
    Ub = csb[:, 0:128]; UTb = csb[:, 128:256]; onesb = csb[:, 256:384]; identb = csb[:, 384:512]
    mnfb = csb[:, 512:640]; mnbb = csb[:, 640:768]
    b_idb = b_csb
    dhf = p.sb("dhf", [128, NCH, 4], F32); dlf = p.sb("dlf", [128, NCH, 4], F32)
    dhb = p.sb("dhb", [128, NCH, 4], BF16); dlb = p.sb("dlb", [128, NCH, 4], BF16)
    cwt = p.sb("cwt", [128, 15], F32); b_cw = Buf("cw")
    cbt = p.sb("cbt", [128, 3], F32); b_cb = Buf("cb")
    prt = p.sb("prt", [128, 10], F32); b_pr = Buf("pr")
    acoef = p.sb("acoef", [128, 4], F32); b_ac = Buf("ac")
    diag = p.sb("diag", [128, 15, 128], BF16); b_dg = Buf("diag")
    raw = p.sb("raw", [128, S + 4], BF16); b_raw = Buf("raw")
    cT = [p.sb("cT%d" % i, [128, S], BF16) for i in range(2)]; b_cT = [Buf("cT0"), Buf("cT1")]
    x_tm = p.sb("x_tm", [128, NCH, 128], BF16); b_xtm = Buf("xtm")
    B_tm = p.sb("B_tm", [128, NCH, 128], BF16); b_btm = Buf("btm")
    dt = p.sb("dt", [128, NCH, 4], F32); b_dt = Buf("dt")
    dta = p.sb("dta", [128, NCH, 4], F32); b_dta = Buf("dta")
    yacc = p.sb("yacc", [128, NCH, 128], F32); b_y = Buf("yacc")

    psC = [p.ps("psC%d" % i, [128, 512]) for i in range(2)]; b_psC = [Buf("psC0"), Buf("psC1")]
    psT = p.ps("psT", [128, 256], BF16); b_psT = Buf("psT")
    psA = [p.ps("psA%d" % i, [128, 512]) for i in range(2)]; b_psA = [Buf("psA0"), Buf("psA1")]
    psS = p.ps("psS", [128, 512]); b_psS = [Buf("psS")] * 4
    psY = [p.ps("psY%d" % i, [128, 512]) for i in range(2)]; _by = [Buf("psY0"), Buf("psY1")]; b_psY = [_by[0], _by[0], _by[1], _by[1]]

    R = [[p.sb("R%d_%d" % (par, u), [128, 2, 128], BF16) for u in range(4)] for par in range(3)]
    b_R = [[Buf("R") for u in range(4)] for par in range(3)]
    DT_ = [[p.sb("D%d_%d" % (par, u), [128, 128], BF16) for u in range(4)] for par in range(3)]
    b_D = [[Buf("D") for u in range(4)] for par in range(3)]
    M = [[p.sb("M%d_%d" % (par, u), [128, 128], BF16) for u in range(4)] for par in range(3)]
    b_M = [[Buf("M") for u in range(4)] for par in range(3)]
    xw = [[p.sb("xw%d_%d" % (par, u), [128, 64], BF16) for u in range(4)] for par in range(3)]
    b_xw = [[Buf("xw") for u in range(4)] for par in range(3)]
    sm = [p.sb("sm%d" % par, [128, 8, 4], F32) for par in range(3)]
    b_sm = [Buf("sm0"), Buf("sm1"), Buf("sm2")]
    yd = [[p.sb("yd%d_%d" % (par, u), [128, 64], F32) for u in range(4)] for par in range(2)]
    b_yd = [[Buf("yd") for u in range(4)] for par in range(2)]
    ytmp = [[p.sb("yt%d_%d" % (par, u), [128, 64], F32) for u in range(4)] for par in range(2)]
    b_yt = [[Buf("yt") for u in range(4)] for par in range(2)]
    hT = [p.sb("hT%d" % u, [128, 64], F32) for u in range(4)]; b_h = [Buf("h%d" % u) for u in range(4)]
    hb = [[p.sb("hb%d_%d" % (par, u), [128, 64], BF16) for u in range(4)] for par in range(2)]
    b_hb = [[Buf("hb") for u in range(4)] for par in range(2)]

    p.dma("sync", cs[:], cst, "cs", writes=[b_cs])
    p.dma("sync", cwt[:], cw, "cw", writes=[b_cw])
    p.dma("sync", cbt[:], cb, "cb", writes=[b_cb])
    p.dma("sync", prt[:], prm, "pr", writes=[b_pr])
    p.dma("sync", dt[:].rearrange("p c k -> p (c k)"), dt_in, "dt", writes=[b_dt])
    p.op("vector", lambda e: e.tensor_copy(out=csb[:], in_=cs[:]), reads=[b_cs], writes=[b_csb])
    for i in range(15):
        p.op("vector", lambda e, i=i: e.tensor_scalar(out=diag[:, i, :], in0=identf, scalar1=cwt[:, i:i + 1], scalar2=None,
                                                      op0=ALU.mult), reads=[b_cs, b_cw], writes=[b_dg])
    p.op("scalar", lambda e: e.activation(out=acoef[:], in_=prt[:, 4:8], func=AF.Exp), reads=[b_pr], writes=[b_ac])
    p.op("vector", lambda e: e.tensor_scalar(out=acoef[:], in0=acoef[:], scalar1=-1.0, scalar2=None, op0=ALU.mult),
         reads=[b_ac], writes=[b_ac])
    for k in range(4):
        p.op("vector", lambda e, k=k: e.tensor_scalar(out=dt[:, :, k], in0=dt[:, :, k], scalar1=prt[:, k:k + 1], scalar2=None,
                                                      op0=ALU.add), reads=[b_dt, b_pr], writes=[b_dt])
    p.op("scalar", lambda e: e.activation(out=dt[:], in_=dt[:], func=AF.Exp), reads=[b_dt], writes=[b_dt])
    p.op("scalar", lambda e: e.activation(out=dt[:], in_=dt[:], func=AF.Ln, bias=ones[:, 0:1], scale=1.0), reads=[b_dt, b_cs], writes=[b_dt])
    for k in range(4):
        p.op("vector", lambda e, k=k: e.tensor_scalar(out=dta[:, :, k], in0=dt[:, :, k], scalar1=acoef[:, k:k + 1], scalar2=None,
                                                      op0=ALU.mult), reads=[b_dt, b_ac], writes=[b_dta])

    p.op("vector", lambda e: e.tensor_copy(out=dhb[:], in_=dta[:]), reads=[b_dta], writes=[b_dta])
    p.op("vector", lambda e: e.tensor_copy(out=dhf[:], in_=dhb[:]), reads=[b_dta], writes=[b_dta])
    p.op("vector", lambda e: e.tensor_tensor(out=dlf[:], in0=dta[:], in1=dhf[:], op=ALU.subtract), reads=[b_dta], writes=[b_dta])
    p.op("vector", lambda e: e.tensor_copy(out=dlb[:], in_=dlf[:]), reads=[b_dta], writes=[b_dta])

    ci = 0
    for chunk, dst in ((0, 0), (1, 1), (2, 0)):
        p.dma("sync", raw[:], raw_in[chunk], "raw", writes=[b_raw])
        for tt in range(S // 512):
            b = ci % 2
            ci += 1
            for k in range(5):
                p.op("tensor", lambda e, b=b, k=k, tt=tt, chunk=chunk: e.matmul(
                    psC[b][:], lhsT=diag[:, chunk * 5 + k, :], rhs=raw[:, tt * 512 + k:tt * 512 + k + 512],
                    start=(k == 0), stop=(k == 4)), reads=[b_dg, b_raw], writes=[b_psC[b]], count=(k == 4))
            p.op("scalar", lambda e, b=b, tt=tt, chunk=chunk, dst=dst: e.activation(
                out=cT[dst][:, tt * 512:(tt + 1) * 512], in_=psC[b][:], func=AF.Silu, bias=cbt[:, chunk:chunk + 1]),
                reads=[b_psC[b], b_cb], writes=[b_cT[dst]])
        if chunk < 2:
            tgt, b_tgt = (x_tm, b_xtm) if chunk == 0 else (B_tm, b_btm)
            for c2 in range(NCH // 2):
                for q in range(2):
                    c = c2 * 2 + q
                    p.op("tensor", lambda e, c=c, q=q, dst=dst: e.transpose(psT[:, q * 128:(q + 1) * 128],
                                                                          cT[dst][:, c * 128:(c + 1) * 128], identb),
                         reads=[b_cT[dst], b_idb], writes=[b_psT], count=(q == 1))
                p.op("vector", lambda e, c2=c2, tgt=tgt: e.tensor_copy(
                    out=tgt[:, 2 * c2:2 * c2 + 2, :], in_=psT[:].rearrange("p (q f) -> p q f", q=2)),
                    reads=[b_psT], writes=[b_tgt])
    BcT = cT[1]; b_B = b_cT[1]
    CcT = cT[0]; b_C = b_cT[0]

    for h in range(2):
        p.op("vector", lambda e, h=h: e.tensor_scalar(out=yacc[:, :, h * 64:(h + 1) * 64], in0=x_tm[:, :, h * 64:(h + 1) * 64],
                                                      scalar1=prt[:, 8 + h:9 + h], scalar2=None, op0=ALU.mult),
             reads=[b_xtm, b_pr], writes=[b_y])
    for u in range(4):
        p.op("gpsimd", lambda e, u=u: e.memset(hT[u][:], 0.0), writes=[b_h[u]])
        p.op("gpsimd", lambda e, u=u: e.memset(hb[1][u][:], 0.0), writes=[b_hb[1][u]])

    def chunk_of(i, d):
        return i if d == 0 else NCH - 1 - i

    psA3 = [psA[0], psA[1], psC[0]]
    b_psA3 = [b_psA[0], b_psA[1], b_psC[0]]

    def front(i):
        par = i % 3
        A = psA3[par]
        for d in range(2):
            c = chunk_of(i, d)
            p.op("tensor", lambda e, d=d, c=c, A=A: e.matmul(A[:, d * 128:(d + 1) * 128], lhsT=BcT[:, c * 128:(c + 1) * 128],
                                                             rhs=CcT[:, c * 128:(c + 1) * 128], start=True, stop=True),
                 reads=[b_B, b_C], writes=[b_psA3[par]], count=False)
            for lhs, off in (((Ub if d == 0 else UTb), 256 + d * 8), (onesb, 260 + d * 8)):
                p.op("tensor", lambda e, c=c, A=A, lhs=lhs, off=off: e.matmul(A[:, off:off + 4], lhsT=lhs, rhs=dhb[:, c, :],
                                                                             start=True, stop=False),
                     reads=[b_csb, b_dta], writes=[b_psA3[par]], count=False)
                p.op("tensor", lambda e, c=c, A=A, lhs=lhs, off=off: e.matmul(A[:, off:off + 4], lhsT=lhs, rhs=dlb[:, c, :],
                                                                             start=False, stop=True),
                     reads=[b_csb, b_dta], writes=[b_psA3[par]], count=(d == 1 and off == 268))
        s_ = sm[par]
        for d in range(2):
            ks = slice(d * 2, d * 2 + 2)
            ac = A[:, 256 + d * 8 + d * 2:256 + d * 8 + d * 2 + 2]
            tt_ = A[:, 260 + d * 8 + d * 2:260 + d * 8 + d * 2 + 2]
            p.op("vector", lambda e, ac=ac, ks=ks: e.tensor_scalar(out=s_[:, 0, ks], in0=ac, scalar1=-1.0, scalar2=None, op0=ALU.mult),
                 reads=[b_psA3[par]], writes=[b_sm[par]])
            p.op("vector", lambda e, ac=ac, tt_=tt_, ks=ks: e.tensor_tensor(out=s_[:, 1, ks], in0=tt_, in1=s_[:, 0, ks], op=ALU.add),
                 reads=[b_psA3[par], b_sm[par]], writes=[b_sm[par]])
            p.op("scalar", lambda e, tt_=tt_, ks=ks: e.activation(out=s_[:, 3, ks], in_=tt_, func=AF.Exp),
                 reads=[b_psA3[par]], writes=[b_sm[par]])
            p.op("scalar", lambda e, ac=ac, ks=ks: e.activation(out=s_[:, 4, ks], in_=ac, func=AF.Exp),
                 reads=[b_psA3[par]], writes=[b_sm[par]])
        p.op("scalar", lambda e: e.activation(out=s_[:, 2, :], in_=s_[:, 1, :], func=AF.Exp), reads=[b_sm[par]], writes=[b_sm[par]])
        for d in range(2):
            c = chunk_of(i, d)
            p.op("vector", lambda e, d=d, c=c: e.tensor_tensor(out=s_[:, 5, d * 2:d * 2 + 2], in0=s_[:, 2, d * 2:d * 2 + 2],
                                                               in1=dt[:, c, d * 2:d * 2 + 2], op=ALU.mult),
                 reads=[b_sm[par], b_dt], writes=[b_sm[par]])
        for u in range(4):
            d, h = u // 2, u % 2
            c = chunk_of(i, d)
            for q, src in ((0, dhf), (1, dlf)):
                p.op("vector", lambda e, u=u, d=d, c=c, q=q, src=src: e.tensor_scalar(
                    out=R[par][u][:, q, :], in0=(U if d == 0 else UT), scalar1=src[:, c, u:u + 1], scalar2=None, op0=ALU.mult),
                    reads=[b_cs, b_dta], writes=[b_R[par][u]])
        for u in range(4):
            d = u // 2
            p.op("tensor", lambda e, u=u: e.matmul(psS[:, u * 128:(u + 1) * 128], lhsT=onesb, rhs=R[par][u][:, 0, :], start=True, stop=False),
                 reads=[b_csb, b_R[par][u]], writes=[b_psS[u]], count=False)
            p.op("tensor", lambda e, u=u: e.matmul(psS[:, u * 128:(u + 1) * 128], lhsT=onesb, rhs=R[par][u][:, 1, :], start=False, stop=False),
                 reads=[b_csb, b_R[par][u]], writes=[b_psS[u]], count=False)
            p.op("tensor", lambda e, u=u, d=d: e.matmul(psS[:, u * 128:(u + 1) * 128], lhsT=identb, rhs=(mnfb if d == 0 else mnbb),
                                                        start=False, stop=True),
                 reads=[b_csb], writes=[b_psS[u]], count=(u == 3))
        for u in range(4):
            p.op("scalar", lambda e, u=u: e.activation(out=DT_[par][u][:], in_=psS[:, u * 128:(u + 1) * 128], func=AF.Exp,
                                                       bias=s_[:, 0, u:u + 1]),
                 reads=[b_psS[u], b_sm[par]], writes=[b_D[par][u]])
        for u in range(4):
            d, h = u // 2, u % 2
            c = chunk_of(i, d)
            p.op("vector", lambda e, u=u, d=d, c=c, A=A: e.scalar_tensor_tensor(
                out=M[par][u][:], in0=A[:, d * 128:(d + 1) * 128], scalar=dt[:, c, u:u + 1], in1=DT_[par][u][:],
                op0=ALU.mult, op1=ALU.mult), reads=[b_psA3[par], b_dt, b_D[par][u]], writes=[b_M[par][u]])
            p.op("vector", lambda e, u=u, h=h, c=c: e.tensor_scalar(out=xw[par][u][:], in0=x_tm[:, c, h * 64:(h + 1) * 64],
                                                                    scalar1=s_[:, 5, u:u + 1], scalar2=None, op0=ALU.mult),
                 reads=[b_xtm, b_sm[par]], writes=[b_xw[par][u]])

    def back(i):
        par = i % 3
        bp = i % 2
        s_ = sm[par]
        for u in range(4):
            d, h = u // 2, u % 2
            c = chunk_of(i, d)
            Y = psY[u // 2]
            o = (u % 2) * 192
            p.op("tensor", lambda e, u=u, h=h, c=c, Y=Y, o=o: e.matmul(Y[:, o:o + 64], lhsT=M[par][u][:],
                                                                      rhs=x_tm[:, c, h * 64:(h + 1) * 64], start=True, stop=True),
                 reads=[b_M[par][u], b_xtm], writes=[b_psY[u]], count=False)
            p.op("tensor", lambda e, u=u, c=c, Y=Y, o=o: e.matmul(Y[:, o + 64:o + 128], lhsT=CcT[:, c * 128:(c + 1) * 128],
                                                                 rhs=hb[1 - bp][u][:], start=True, stop=True),
                 reads=[b_C, b_hb[1 - bp][u]], writes=[b_psY[u]], count=False)
            p.op("tensor", lambda e, u=u, c=c, Y=Y, o=o: e.matmul(Y[:, o + 128:o + 192], lhsT=B_tm[:, c, :],
                                                                 rhs=xw[par][u][:], start=True, stop=True),
                 reads=[b_btm, b_xw[par][u]], writes=[b_psY[u]], count=(u % 2 == 1))
        for u in range(4):
            d, h = u // 2, u % 2
            c = chunk_of(i, d)
            Y = psY[u // 2]
            o = (u % 2) * 192
            p.op("vector", lambda e, u=u, Y=Y, o=o: e.scalar_tensor_tensor(out=hT[u][:], in0=hT[u][:], scalar=s_[:, 3, u:u + 1],
                                                                          in1=Y[:, o + 128:o + 192], op0=ALU.mult, op1=ALU.add),
                 reads=[b_h[u], b_sm[par], b_psY[u]], writes=[b_h[u]])
            p.op("scalar", lambda e, u=u: e.copy(out=hb[bp][u][:], in_=hT[u][:]), reads=[b_h[u]], writes=[b_hb[bp][u]])
            p.op("scalar", lambda e, u=u, Y=Y, o=o: e.copy(out=yd[bp][u][:], in_=Y[:, o:o + 64]),
                 reads=[b_psY[u]], writes=[b_yd[bp][u]])
            p.op("vector", lambda e, u=u, Y=Y, o=o: e.scalar_tensor_tensor(out=ytmp[bp][u][:], in0=Y[:, o + 64:o + 128],
                                                                          scalar=s_[:, 4, u:u + 1], in1=yd[bp][u][:],
                                                                          op0=ALU.mult, op1=ALU.add),
                 reads=[b_psY[u], b_sm[par], b_yd[bp][u]], writes=[b_yt[bp][u]])
            p.op("vector", lambda e, u=u, h=h, c=c: e.tensor_tensor(out=yacc[:, c, h * 64:(h + 1) * 64],
                                                                    in0=yacc[:, c, h * 64:(h + 1) * 64], in1=ytmp[bp][u][:],
                                                                    op=ALU.add),
                 reads=[b_yt[bp][u], b_y], writes=[b_y])

    front(0)
    front(1)
    for i in range(NCH):
        if i + 2 < NCH:
            front(i + 2)
        back(i)
    p.dma("sync", y_out.rearrange("(c l) f -> l c f", l=128), yacc[:], "yout", reads=[b_y])
    return p.finish()


def k2c_consts():
    U = np.triu(np.ones((128, 128), np.float32))
    UT = np.tril(np.ones((128, 128), np.float32))
    ones = np.ones((128, 128), np.float32)
    ident = np.eye(128, dtype=np.float32)
    mnf = (1 - U) * NEG
    mnb = (1 - UT) * NEG
    return np.concatenate([U, UT, ones, ident, mnf, mnb], axis=1).astype(np.float32)


def k2c_inputs(b, j, ufm, udt, z, L):
    ts = slice(b * S, (b + 1) * S)
    g = j // 2
    XBC0 = (8 + 5 + 4) * 128
    rows = [XBC0 + j * 128, XBC0 + 512 + g * 128, XBC0 + 768 + g * 128]
    raw = np.zeros((3, 128, S + 4), NPBF)
    for i, r0 in enumerate(rows):
        raw[i, :, 2:S + 2] = ufm[r0:r0 + 128, ts]
    cwf = z["ssm_conv_w"][L]
    cbf = z["ssm_conv_b"][L]
    chs = [slice(j * 128, (j + 1) * 128), slice(512 + g * 128, 512 + (g + 1) * 128), slice(768 + g * 128, 768 + (g + 1) * 128)]
    cw = np.concatenate([cwf[:, ch].T for ch in chs], axis=1)
    cb = np.stack([cbf[ch] for ch in chs], axis=1)
    cols = [0 * 8 + 2 * j, 0 * 8 + 2 * j + 1, 1 * 8 + 2 * j, 1 * 8 + 2 * j + 1]
    d4 = udt[ts][:, cols]
    dt_in = np.ascontiguousarray(d4.reshape(NCH, 128, 4).transpose(1, 0, 2)).reshape(128, NCH * 4)
    prm = np.zeros((128, 10), np.float32)
    prm[:, 0:4] = z["ssm_dt_bias"][L].reshape(16)[cols][None, :]
    prm[:, 4:8] = z["ssm_a_log"][L].reshape(16)[cols][None, :]
    prm[:, 8:10] = z["ssm_d"][L][2 * j:2 * j + 2][None, :]
    return {"raw_in": raw, "cw": np.ascontiguousarray(cw), "cb": np.ascontiguousarray(cb), "dt_in": dt_in, "prm": prm,
            "cst": k2c_consts()}


D = 2048
KC = 16
EPS = 1e-6
TT = 256


def build_k3(T=2048):
    p = Prog()
    NT = T // TT
    xT = p.dram("xT", [D, T], F32, "ExternalInput")
    oabT = p.dram("oabT", [1024, T], BF16, "ExternalInput")
    yT = p.dram("yT", [512, T], F32, "ExternalInput")
    zT = p.dram("zT", [512, T], BF16, "ExternalInput")
    confT = p.dram("confT", [1024, T + 30], BF16, "ExternalInput")
    prm = p.dram("prm", [128, 4 * 5 + 16], F32, "ExternalInput")
    dww = p.dram("dww", [128, 4 * 31], F32, "ExternalInput")
    w_out = p.dram("w_out", [D, D], F32, "ExternalInput")
    w_r = p.dram("w_r", [128, KC * 16], F32, "ExternalInput")
    identd = p.dram("identd", [128, 128], F32, "ExternalInput")
    x1T = p.dram("x1T", [D, T], F32, "ExternalOutput")
    hT = p.dram("hT", [D, T], BF16, "ExternalOutput")
    aff = p.dram("aff", [T, 16], F32, "ExternalOutput")

    wo = p.sb("wo", [128, KC, D], BF16); b_wo = Buf("wo")
    prt = p.sb("prt", [128, 36], F32); b_pr = Buf("pr")
    dwt = p.sb("dwt", [128, 124], F32); b_dw = Buf("dw")
    idf = p.sb("idf", [128, 128], F32); b_id = Buf("id")
    diag = p.sb("diag", [128, 124, 128], BF16); b_dg = Buf("dg")
    ones = p.sb("ones", [128, 128], BF16); b_on = Buf("ones")
    epst = p.sb("epst", [128, 1], F32); b_eps = Buf("eps")
    wrf = p.sb("wrf", [128, KC * 16], F32); b_wr = Buf("wr")
    wrh = p.sb("wrh", [128, KC * 16], BF16)
    wrhf = p.sb("wrhf", [128, KC * 16], F32)
    wrl = p.sb("wrl", [128, KC * 16], BF16)

    xt = p.sb("xt", [128, KC, TT], F32); b_xt = Buf("xt")
    mix = p.sb("mix", [128, KC, TT], BF16); b_mix = [Buf("mix%d" % i) for i in range(4)]
    yt = p.sb("yt", [128, 4, TT], F32); b_yt = Buf("yt")
    zt = p.sb("zt", [128, 4, TT], BF16); b_zt = Buf("zt")
    gt = p.sb("gt", [128, 4, TT], F32); b_gt = Buf("gt")
    sq = p.sb("sq", [128, KC, TT], BF16); b_sq = Buf("sq")
    rstd = p.sb("rstd", [128, TT], F32); b_rstd = Buf("rstd")
    cf = p.sb("cf", [128, 8, TT + 30], BF16); b_cf = Buf("cf")
    sg = p.sb("sg", [128, 4, TT + 30], BF16); b_sg = Buf("sg")
    glu = p.sb("glu", [128, 4, TT + 30], BF16); b_glu = Buf("glu")
    cv = p.sb("cv", [128, 4, TT], F32); b_cv = Buf("cv")
    cvb = p.sb("cvb", [128, 4, TT], BF16); b_cvb = Buf("cvb")
    mean = p.sb("mean", [128, TT], F32); b_mean = Buf("mean")
    var = p.sb("var", [128, TT], F32); b_var = Buf("var")
    t1 = p.sb("t1", [128, 4, TT], F32); b_t1 = Buf("t1")
    hf = p.sb("hf", [128, KC, TT], F32); b_hf = Buf("hf")
    hh = p.sb("hh", [128, KC, TT], BF16); b_hh = Buf("hh")
    hl = p.sb("hl", [128, KC, TT], BF16); b_hl = Buf("hl")
    lg = p.sb("lg", [128, 2, 16], F32); b_lg = Buf("lg")
    smx = p.sb("smx", [128, 2, 4], F32); b_smx = Buf("smx")

    psn = p.ps("psn", [128, 512]); b_psn = Buf("psn")
    psc = [p.ps("psc%d" % i, [128, 512]) for i in range(2)]; b_psc = [Buf("psc0"), Buf("psc1")]
    psm = [p.ps("psm%d" % i, [128, 512]) for i in range(3)]; b_psm = [Buf("psm%d" % i) for i in range(3)]
    psr = p.ps("psr", [128, 512]); b_psr = Buf("psr")

    p.dma("sync", prt[:], prm, "pr", writes=[b_pr])
    p.dma("sync", dwt[:], dww, "dw", writes=[b_dw])
    p.dma("sync", idf[:], identd, "id", writes=[b_id])
    p.dma("sync", wrf[:], w_r, "wr", writes=[b_wr])
    p.op("gpsimd", lambda e: e.memset(ones[:], 1.0), writes=[b_on])
    p.op("gpsimd", lambda e: e.memset(epst[:], EPS), writes=[b_eps])
    w_v = w_out.rearrange("(k p) n -> p k n", p=128)
    for q in range(4):
        p.dma("gpsimd", wo[:, :, q * 512:(q + 1) * 512], w_v[:, :, q * 512:(q + 1) * 512], "wo", writes=[b_wo])
    for i in range(124):
        p.op("vector", lambda e, i=i: e.tensor_scalar(out=diag[:, i, :], in0=idf[:], scalar1=dwt[:, i:i + 1], scalar2=None,
                                                      op0=ALU.mult), reads=[b_id, b_dw], writes=[b_dg])
    p.op("vector", lambda e: e.tensor_copy(out=wrh[:], in_=wrf[:]), reads=[b_wr], writes=[b_wr])
    p.op("vector", lambda e: e.tensor_copy(out=wrhf[:], in_=wrh[:]), reads=[b_wr], writes=[b_wr])
    p.op("vector", lambda e: e.tensor_tensor(out=wrhf[:], in0=wrf[:], in1=wrhf[:], op=ALU.subtract), reads=[b_wr], writes=[b_wr])
    p.op("vector", lambda e: e.tensor_copy(out=wrl[:], in_=wrhf[:]), reads=[b_wr], writes=[b_wr])

    xT_v = xT.rearrange("(k p) t -> p k t", p=128)
    oab_v = oabT.rearrange("(k p) t -> p k t", p=128)
    yT_v = yT.rearrange("(k p) t -> p k t", p=128)
    zT_v = zT.rearrange("(k p) t -> p k t", p=128)
    cf_v = confT.rearrange("(k p) t -> p k t", p=128)
    x1_v = x1T.rearrange("(k p) t -> p k t", p=128)
    hT_v = hT.rearrange("(k p) t -> p k t", p=128)

    mi = 0
    for tt in range(NT):
        ts = slice(tt * TT, (tt + 1) * TT)
        p.dma("sync", xt[:], xT_v[:, :, ts], "xt", writes=[b_xt])
        p.dma("sync", mix[:, 0:8, :], oab_v[:, :, ts], "mix", writes=[b_mix[0], b_mix[1]])
        p.dma("sync", yt[:], yT_v[:, :, ts], "yt", writes=[b_yt])
        p.dma("sync", zt[:], zT_v[:, :, ts], "zt", writes=[b_zt])
        p.dma("sync", cf[:], cf_v[:, :, tt * TT:tt * TT + TT + 30], "cf", writes=[b_cf])

        p.op("scalar", lambda e: e.activation(out=gt[:], in_=zt[:], func=AF.Silu), reads=[b_zt], writes=[b_gt])
        p.op("vector", lambda e: e.tensor_tensor(out=gt[:], in0=gt[:], in1=yt[:], op=ALU.mult), reads=[b_gt, b_yt], writes=[b_gt])
        p.op("scalar", lambda e: e.activation(out=sq[:, 0:4, :], in_=gt[:], func=AF.Square), reads=[b_gt], writes=[b_sq])
        for k in range(4):
            p.op("tensor", lambda e, k=k: e.matmul(psn[:, 0:TT], lhsT=ones[:], rhs=sq[:, k, :], start=(k == 0), stop=(k == 3)),
                 reads=[b_on, b_sq], writes=[b_psn], count=(k == 3))
        p.op("scalar", lambda e: e.activation(out=rstd[:], in_=psn[:, 0:TT], func=AF.Sqrt, bias=epst[:], scale=1.0 / 512),
             reads=[b_psn, b_eps], writes=[b_rstd])
        p.op("vector", lambda e: e.reciprocal(out=rstd[:], in_=rstd[:]), reads=[b_rstd], writes=[b_rstd])
        for k in range(4):
            p.op("vector", lambda e, k=k: e.scalar_tensor_tensor(out=mix[:, 8 + k, :], in0=gt[:, k, :], scalar=prt[:, k:k + 1],
                                                                 in1=rstd[:], op0=ALU.mult, op1=ALU.mult),
                 reads=[b_gt, b_pr, b_rstd], writes=[b_mix[2]])

        p.op("scalar", lambda e: e.activation(out=sg[:], in_=cf[:, 4:8, :], func=AF.Sigmoid), reads=[b_cf], writes=[b_sg])
        p.op("vector", lambda e: e.tensor_tensor(out=glu[:], in0=cf[:, 0:4, :], in1=sg[:], op=ALU.mult),
             reads=[b_cf, b_sg], writes=[b_glu])
        for k in range(4):
            b = k % 2
            for j in range(31):
                p.op("tensor", lambda e, k=k, j=j, b=b: e.matmul(psc[b][:, 0:TT], lhsT=diag[:, k * 31 + j, :],
                                                                 rhs=glu[:, k, j:j + TT], start=(j == 0), stop=(j == 30)),
                     reads=[b_dg, b_glu], writes=[b_psc[b]], count=(j == 30))
            p.op("scalar", lambda e, k=k, b=b: e.activation(out=cv[:, k, :], in_=psc[b][:, 0:TT], func=AF.Identity,
                                                            bias=prt[:, 4 + k:5 + k]),
                 reads=[b_psc[b], b_pr], writes=[b_cv])
        p.op("vector", lambda e: e.tensor_copy(out=cvb[:], in_=cv[:]), reads=[b_cv], writes=[b_cvb])
        p.op("scalar", lambda e: e.activation(out=sq[:, 4:8, :], in_=cv[:], func=AF.Square), reads=[b_cv], writes=[b_sq])
        for k in range(4):
            p.op("tensor", lambda e, k=k: e.matmul(psn[:, 0:TT], lhsT=ones[:], rhs=cvb[:, k, :], start=(k == 0), stop=(k == 3)),
                 reads=[b_on, b_cvb], writes=[b_psn], count=False)
        for k in range(4):
            p.op("tensor", lambda e, k=k: e.matmul(psn[:, TT:2 * TT], lhsT=ones[:], rhs=sq[:, 4 + k, :], start=(k == 0), stop=(k == 3)),
                 reads=[b_on, b_sq], writes=[b_psn], count=(k == 3))
        p.op("vector", lambda e: e.tensor_scalar(out=mean[:], in0=psn[:, 0:TT], scalar1=1.0 / 512, scalar2=None, op0=ALU.mult),
             reads=[b_psn], writes=[b_mean])
        p.op("vector", lambda e: e.tensor_tensor(out=var[:], in0=mean[:], in1=mean[:], op=ALU.mult), reads=[b_mean], writes=[b_var])
        p.op("vector", lambda e: e.scalar_tensor_tensor(out=var[:], in0=psn[:, TT:2 * TT], scalar=1.0 / 512, in1=var[:],
                                                        op0=ALU.mult, op1=ALU.subtract),
             reads=[b_psn, b_var], writes=[b_var])
        p.op("scalar", lambda e: e.activation(out=var[:], in_=var[:], func=AF.Sqrt, bias=epst[:], scale=1.0),
             reads=[b_var, b_eps], writes=[b_var])
        p.op("vector", lambda e: e.reciprocal(out=var[:], in_=var[:]), reads=[b_var], writes=[b_var])
        for k in range(4):
            p.op("vector", lambda e, k=k: e.tensor_tensor(out=t1[:, k, :], in0=cv[:, k, :], in1=mean[:], op=ALU.subtract),
                 reads=[b_cv, b_mean], writes=[b_t1])
            p.op("vector", lambda e, k=k: e.tensor_tensor(out=t1[:, k, :], in0=t1[:, k, :], in1=var[:], op=ALU.mult),
                 reads=[b_t1, b_var], writes=[b_t1])
            p.op("scalar", lambda e, k=k: e.activation(out=mix[:, 12 + k, :], in_=t1[:, k, :], func=AF.Silu,
                                                       bias=prt[:, 12 + k:13 + k], scale=prt[:, 8 + k:9 + k]),
                 reads=[b_t1, b_pr], writes=[b_mix[3]])

        for dc in range(KC):
            b = mi % 3
            mi += 1
            for k in range(KC):
                p.op("tensor", lambda e, dc=dc, k=k, b=b: e.matmul(psm[b][:, 0:TT], lhsT=wo[:, k, dc * 128:(dc + 1) * 128],
                                                                   rhs=mix[:, k, :], start=(k == 0), stop=(k == KC - 1)),
                     reads=[b_wo] + b_mix, writes=[b_psm[b]], count=(k == KC - 1))
            p.op("vector", lambda e, dc=dc, b=b: e.tensor_tensor(out=xt[:, dc, :], in0=xt[:, dc, :], in1=psm[b][:, 0:TT], op=ALU.add),
                 reads=[b_psm[b], b_xt], writes=[b_xt])
        p.dma("sync", x1_v[:, :, ts], xt[:], "x1o", reads=[b_xt])

        p.op("scalar", lambda e: e.activation(out=sq[:], in_=xt[:], func=AF.Square), reads=[b_xt], writes=[b_sq])
        for k in range(KC):
            p.op("tensor", lambda e, k=k: e.matmul(psn[:, 0:TT], lhsT=ones[:], rhs=sq[:, k, :], start=(k == 0), stop=(k == KC - 1)),
                 reads=[b_on, b_sq], writes=[b_psn], count=(k == KC - 1))
        p.op("scalar", lambda e: e.activation(out=rstd[:], in_=psn[:, 0:TT], func=AF.Sqrt, bias=epst[:], scale=1.0 / D),
             reads=[b_psn, b_eps], writes=[b_rstd])
        p.op("vector", lambda e: e.reciprocal(out=rstd[:], in_=rstd[:]), reads=[b_rstd], writes=[b_rstd])
        for k in range(KC):
            p.op("vector", lambda e, k=k: e.scalar_tensor_tensor(out=hf[:, k, :], in0=xt[:, k, :], scalar=prt[:, 20 + k:21 + k],
                                                                 in1=rstd[:], op0=ALU.mult, op1=ALU.mult),
                 reads=[b_xt, b_pr, b_rstd], writes=[b_hf])
        p.op("scalar", lambda e: e.copy(out=hh[:], in_=hf[:]), reads=[b_hf], writes=[b_hh])
        p.op("vector", lambda e: e.tensor_tensor(out=hf[:], in0=hf[:], in1=hh[:], op=ALU.subtract), reads=[b_hf, b_hh], writes=[b_hf])
        p.op("scalar", lambda e: e.copy(out=hl[:], in_=hf[:]), reads=[b_hf], writes=[b_hl])
        p.dma("sync", hT_v[:, :, ts], hh[:], "ho", reads=[b_hh])
        for s2 in range(TT // 128):
            first = True
            for k in range(KC):
                for (a_, w_) in ((hh, wrh), (hh, wrl), (hl, wrh)):
                    last = (k == KC - 1 and a_ is hl)
                    p.op("tensor", lambda e, s2=s2, k=k, a_=a_, w_=w_, first=first, last=last: e.matmul(
                        psr[:, s2 * 16:(s2 + 1) * 16], lhsT=a_[:, k, s2 * 128:(s2 + 1) * 128], rhs=w_[:, k * 16:(k + 1) * 16],
                        start=first, stop=last), reads=[b_hh, b_hl, b_wr], writes=[b_psr], count=(last and s2 == TT // 128 - 1))
                    first = False
        p.op("vector", lambda e: e.tensor_copy(out=lg[:], in_=psr[:, 0:32].rearrange("p (s e) -> p s e", s=2)),
             reads=[b_psr], writes=[b_lg])
        for s2 in range(TT // 128):
            p.op("vector", lambda e, s2=s2: e.reduce_max(out=smx[:, s2, 0:1], in_=lg[:, s2, :], axis=AX.X), reads=[b_lg], writes=[b_smx])
            p.op("vector", lambda e, s2=s2: e.tensor_scalar(out=smx[:, s2, 1:2], in0=smx[:, s2, 0:1], scalar1=-1.0, scalar2=None,
                                                            op0=ALU.mult), reads=[b_smx], writes=[b_smx])
            p.op("scalar", lambda e, s2=s2: e.activation(out=lg[:, s2, :], in_=lg[:, s2, :], func=AF.Exp, bias=smx[:, s2, 1:2]),
                 reads=[b_lg, b_smx], writes=[b_lg])
            p.op("vector", lambda e, s2=s2: e.reduce_sum(out=smx[:, s2, 2:3], in_=lg[:, s2, :], axis=AX.X), reads=[b_lg], writes=[b_smx])
            p.op("vector", lambda e, s2=s2: e.reciprocal(out=smx[:, s2, 3:4], in_=smx[:, s2, 2:3]), reads=[b_smx], writes=[b_smx])
            p.op("vector", lambda e, s2=s2: e.tensor_scalar(out=lg[:, s2, :], in0=lg[:, s2, :], scalar1=smx[:, s2, 3:4], scalar2=None,
                                                            op0=ALU.mult), reads=[b_lg, b_smx], writes=[b_lg])
        p.dma("sync", aff[tt * TT:(tt + 1) * TT, :].rearrange("(s p) e -> p s e", p=128), lg[:], "affo", reads=[b_lg])
    return p.finish()


def k3_inputs(c, x_tm, o_na, o_wa, y, ufm, z, L):
    T = 2048
    ts = slice(c * T, (c + 1) * T)
    b = c // 4
    Z0 = (8 + 5) * 128
    CF0 = (8 + 5 + 12) * 128
    conf = np.zeros((1024, T + 30), NPBF)
    lo = c * T - 15
    hi = c * T + T + 15
    slo = max(lo, b * 8192)
    shi = min(hi, (b + 1) * 8192)
    conf[:, slo - lo:slo - lo + (shi - slo)] = ufm[CF0:CF0 + 1024, slo:shi]
    prm = np.zeros((128, 36), np.float32)
    prm[:, 0:4] = z["ssm_norm_w"][L].reshape(4, 128).T
    prm[:, 4:8] = z["conf_dw_b"][L].reshape(4, 128).T
    prm[:, 8:12] = z["conf_ln_w"][L].reshape(4, 128).T
    prm[:, 12:16] = z["conf_ln_b"][L].reshape(4, 128).T
    prm[:, 20:36] = z["ffn_norm_w"][L].reshape(16, 128).T
    dww = np.ascontiguousarray(z["conf_dw_w"][L].reshape(31, 4, 128).transpose(2, 1, 0)).reshape(128, 124)
    w_r = np.ascontiguousarray(z["w_router"][L].reshape(16, 128, 16).transpose(1, 0, 2)).reshape(128, 256)
    return {
        "xT": np.ascontiguousarray(x_tm[ts].T),
        "oabT": np.ascontiguousarray(np.concatenate([o_na[ts], o_wa[ts]], axis=1).T),
        "yT": np.ascontiguousarray(y[ts].T),
        "zT": np.ascontiguousarray(ufm[Z0:Z0 + 512, ts]),
        "confT": conf, "prm": prm, "dww": dww, "w_out": z["w_out"][L], "w_r": w_r,
        "identd": np.eye(128, dtype=np.float32),
    }


D = 2048
FF = 1024
NIT = 40


def build_k4(S=8192, CAP=1024):
    p = Prog()
    NJ = S // 128
    NSH = CAP // 512
    affp = p.dram("affp", [128, 4 * NJ], F32, "ExternalInput")
    h_tmb = [p.dram("h_tm%d" % b_, [S, D], BF16, "ExternalInput") for b_ in range(2)]
    wg = p.dram("wg", [2, D, FF], F32, "ExternalInput")
    wu = p.dram("wu", [2, D, FF], F32, "ExternalInput")
    wd = p.dram("wd", [2, FF, D], F32, "ExternalInput")
    cst = p.dram("cst", [128, 128 + CAP], F32, "ExternalInput")
    cst2 = p.dram("cst2", [128, 128 + 2 * NJ], F32, "ExternalInput")
    y_out = p.dram("y_out", [4, CAP, D], BF16, "ExternalOutput")
    slot_out = p.dram("slot_out", [128, 4 * NJ], F32, "ExternalOutput")

    csf = p.sb("csf", [128, 128 + CAP], F32); b_cs = Buf("cs")
    iota = csf[:, 128:128 + CAP]
    utb = p.sb("utb", [128, 128], BF16); b_ut = Buf("ut")
    onesb = p.sb("onesb", [128, 128], BF16); b_on = Buf("ones")
    aff3 = p.sb("aff3", [128, 4, NJ], F32); b_aff = Buf("aff")
    cmp_ = p.sb("cmp", [128, 4, NJ], F32); b_cmp = Buf("cmp")
    cmpb = p.sb("cmpb", [128, 4, NJ], BF16); b_cmpb = Buf("cmpb")
    sc1 = p.sb("sc1", [128, 4, NJ], F32); b_sc1 = Buf("sc1")
    sc2 = p.sb("sc2", [128, 4, NJ], F32); b_sc2 = Buf("sc2")
    slotc = p.sb("slotc", [128, 4, NJ], F32); b_slot = Buf("slot")
    sm = p.sb("sm", [128, 8, 4], F32); b_sm = Buf("sm")
    cntb = p.sb("cntb", [128, 4], BF16); b_cntb = Buf("cntb")
    ps = [p.ps("ps%d" % i, [128, 512]) for i in range(6)]; b_ps = [Buf("ps%d" % i) for i in range(6)]
    psT = [p.ps("psT%d" % i, [128, 512], BF16) for i in range(2)]; b_psT = [Buf("psT0"), Buf("psT1")]

    c2f = p.sb("c2f", [128, 128 + 2 * NJ], F32); b_c2 = Buf("c2")
    c2b = p.sb("c2b", [128, 128 + 2 * NJ], BF16)
    identb = c2b[:, 0:128]
    jp = c2b[:, 128:128 + 2 * NJ]
    p.dma("sync", c2f[:], cst2, "c2", writes=[b_c2])
    p.op("vector", lambda e: e.tensor_copy(out=c2b[:], in_=c2f[:]), reads=[b_c2], writes=[b_c2])
    p.dma("sync", csf[:], cst, "cs", writes=[b_cs])
    p.dma("sync", aff3[:].rearrange("p i j -> p (i j)"), affp, "aff", writes=[b_aff])
    p.op("vector", lambda e: e.tensor_copy(out=utb[:], in_=csf[:, 0:128]), reads=[b_cs], writes=[b_ut])
    p.op("gpsimd", lambda e: e.memset(onesb[:], 1.0), writes=[b_on])
    p.op("gpsimd", lambda e: e.memset(sm[:], 0.0), writes=[b_sm])
    p.op("gpsimd", lambda e: e.memset(sm[:, 1, :], 1.0), reads=[b_sm], writes=[b_sm])

    lo = sm[:, 0, :]; hi = sm[:, 1, :]; mid = sm[:, 2, :]; cpart = sm[:, 3, :]; ge = sm[:, 4, :]; dd = sm[:, 5, :]
    V = "vector"

    def count_ge(thr):
        for i in range(4):
            p.op(V, lambda e, i=i: e.tensor_scalar(out=cmp_[:, i, :], in0=aff3[:, i, :], scalar1=thr[:, i:i + 1], scalar2=None,
                                                   op0=ALU.is_ge), reads=[b_aff, b_sm], writes=[b_cmp])

    for it in range(NIT):
        p.op(V, lambda e: e.tensor_tensor(out=mid, in0=lo, in1=hi, op=ALU.add), reads=[b_sm], writes=[b_sm])
        p.op(V, lambda e: e.tensor_scalar(out=mid, in0=mid, scalar1=0.5, scalar2=None, op0=ALU.mult), reads=[b_sm], writes=[b_sm])
        count_ge(mid)
        p.op(V, lambda e: e.reduce_sum(out=cpart, in_=cmp_[:], axis=AX.X), reads=[b_cmp], writes=[b_sm])
        p.op(V, lambda e: e.tensor_copy(out=cntb[:], in_=cpart), reads=[b_sm], writes=[b_cntb])
        p.op("tensor", lambda e: e.matmul(ps[0][:, 0:4], lhsT=onesb[:], rhs=cntb[:], start=True, stop=True),
             reads=[b_on, b_cntb], writes=[b_ps[0]])
        p.op(V, lambda e: e.tensor_scalar(out=ge, in0=ps[0][:, 0:4], scalar1=float(CAP), scalar2=None, op0=ALU.is_ge),
             reads=[b_ps[0]], writes=[b_sm])
        p.op(V, lambda e: e.tensor_tensor(out=dd, in0=mid, in1=lo, op=ALU.subtract), reads=[b_sm], writes=[b_sm])
        p.op(V, lambda e: e.tensor_tensor(out=dd, in0=dd, in1=ge, op=ALU.mult), reads=[b_sm], writes=[b_sm])
        p.op(V, lambda e: e.tensor_tensor(out=lo, in0=lo, in1=dd, op=ALU.add), reads=[b_sm], writes=[b_sm])
        p.op(V, lambda e: e.tensor_tensor(out=dd, in0=hi, in1=mid, op=ALU.subtract), reads=[b_sm], writes=[b_sm])
        p.op(V, lambda e: e.tensor_tensor(out=dd, in0=dd, in1=ge, op=ALU.mult), reads=[b_sm], writes=[b_sm])
        p.op(V, lambda e: e.tensor_tensor(out=hi, in0=mid, in1=dd, op=ALU.add), reads=[b_sm], writes=[b_sm])

    count_ge(lo)
    p.op(V, lambda e: e.tensor_copy(out=cmpb[:], in_=cmp_[:]), reads=[b_cmp], writes=[b_cmpb])
    cflat = cmpb[:].rearrange("p i j -> p (i j)")
    p.op("tensor", lambda e: e.matmul(ps[1][:, 0:4 * NJ], lhsT=utb[:], rhs=cflat, start=True, stop=True),
         reads=[b_ut, b_cmpb], writes=[b_ps[1]])
    p.op("tensor", lambda e: e.matmul(ps[2][:, 0:4 * NJ], lhsT=onesb[:], rhs=cflat, start=True, stop=True),
         reads=[b_on, b_cmpb], writes=[b_ps[2]])
    p.op(V, lambda e: e.tensor_copy(out=sc1[:].rearrange("p i j -> p (i j)"), in_=ps[2][:, 0:4 * NJ]), reads=[b_ps[2]], writes=[b_sc1])
    src, b_src, dst, b_dst = sc1, b_sc1, sc2, b_sc2
    s = 1
    while s < NJ:
        p.op(V, lambda e, s=s, src=src, dst=dst: e.tensor_tensor(out=dst[:, :, s:], in0=src[:, :, s:], in1=src[:, :, 0:NJ - s], op=ALU.add),
             reads=[b_src], writes=[b_dst])
        p.op(V, lambda e, s=s, src=src, dst=dst: e.tensor_copy(out=dst[:, :, 0:s], in_=src[:, :, 0:s]), reads=[b_src], writes=[b_dst])
        src, b_src, dst, b_dst = dst, b_dst, src, b_src
        s *= 2
    incl, b_incl = src, b_src
    p.op(V, lambda e: e.tensor_tensor(out=slotc[:].rearrange("p i j -> p (i j)"), in0=incl[:].rearrange("p i j -> p (i j)"),
                                      in1=ps[2][:, 0:4 * NJ], op=ALU.subtract), reads=[b_incl, b_ps[2]], writes=[b_slot])
    p.op(V, lambda e: e.tensor_tensor(out=slotc[:].rearrange("p i j -> p (i j)"), in0=slotc[:].rearrange("p i j -> p (i j)"),
                                      in1=ps[1][:, 0:4 * NJ], op=ALU.add), reads=[b_slot, b_ps[1]], writes=[b_slot])
    p.op(V, lambda e: e.tensor_tensor(out=slotc[:], in0=slotc[:], in1=cmp_[:], op=ALU.mult), reads=[b_slot, b_cmp], writes=[b_slot])
    p.op(V, lambda e: e.tensor_scalar(out=slotc[:], in0=slotc[:], scalar1=-1.0, scalar2=None, op0=ALU.add), reads=[b_slot], writes=[b_slot])
    p.dma("sync", slot_out, slotc[:].rearrange("p i j -> p (i j)"), "slo", reads=[b_slot])

    xinT = p.sb("xinT", [128, 16, CAP], BF16); b_xin = Buf("xin")
    hidT = p.sb("hidT", [128, 8, CAP], BF16); b_hid = Buf("hid")
    wdt = p.sb("wdt", [128, 8, D], BF16); b_wd = Buf("wd")
    wgt = [p.sb("wgt%d" % i, [128, 16, 128], BF16) for i in range(2)]; b_wg = [Buf("wg0"), Buf("wg1")]
    wut = [p.sb("wut%d" % i, [128, 16, 128], BF16) for i in range(2)]; b_wu = [Buf("wu0"), Buf("wu1")]
    sel = [p.sb("sel%d" % i, [128, CAP], BF16) for i in range(3)]; b_sel = [Buf("sel%d" % i) for i in range(3)]
    act = [p.sb("act%d" % i, [128, 512], F32) for i in range(2)]; b_act = [Buf("a0"), Buf("a1")]
    yb = [p.sb("yb%d" % i, [128, D], BF16) for i in range(2)]; b_yb = [Buf("y0"), Buf("y1")]
    xg = [p.sb("xg%d" % i, [128, D], BF16) for i in range(2)]; b_xg = [Buf("xg0"), Buf("xg1")]
    NSC = CAP // 128
    idxf = p.sb("idxf", [128, NSC, 2], F32); b_idxf = Buf("idxf")
    idx1 = p.sb("idx1", [128, NSC], F32); b_idx1 = Buf("idx1")
    idxi = p.sb("idxi", [128, NSC], I32); b_idxi = Buf("idxi")
    n = {"s": 0, "ev": 0, "w": 0, "a": 0, "y": 0, "g": 0, "t": 0}
    for i in range(4):
        b, el = i // 2, i % 2
        for j in range(NJ):
            ss = n["s"] % 3; n["s"] += 1
            p.op(V, lambda e, ss=ss, i=i, j=j: e.tensor_scalar(out=sel[ss][:], in0=iota, scalar1=slotc[:, i, j:j + 1], scalar2=None,
                                                              op0=ALU.is_equal), reads=[b_cs, b_slot], writes=[b_sel[ss]])
            for sc in range(NSC):
                p.op("tensor", lambda e, ss=ss, sc=sc, j=j: e.matmul(ps[0][:, sc * 2:sc * 2 + 2], lhsT=sel[ss][:, sc * 128:(sc + 1) * 128],
                                                                    rhs=jp[:, 2 * j:2 * j + 2], start=(j == 0 and sc == 0),
                                                                    stop=(j == NJ - 1 and sc == NSC - 1)),
                     reads=[b_sel[ss], b_c2], writes=[b_ps[0]], count=(sc == NSC - 1))
        p.op(V, lambda e: e.tensor_copy(out=idxf[:].rearrange("p s t -> p (s t)"), in_=ps[0][:, 0:2 * NSC]), reads=[b_ps[0]], writes=[b_idxf])
        p.op(V, lambda e: e.scalar_tensor_tensor(out=idx1[:], in0=idxf[:, :, 0], scalar=128.0, in1=idxf[:, :, 1], op0=ALU.mult, op1=ALU.add),
             reads=[b_idxf], writes=[b_idx1])
        p.op(V, lambda e: e.tensor_copy(out=idxi[:], in_=idx1[:]), reads=[b_idx1], writes=[b_idxi])
        for sc in range(NSC):
            gs = n["g"] % 2; n["g"] += 1
            p.raw("gpsimd", lambda e, gs=gs, sc=sc, b=b: e.indirect_dma_start(
                out=xg[gs][:], out_offset=None, in_=h_tmb[b][:, :],
                in_offset=bass.IndirectOffsetOnAxis(ap=idxi[:, sc:sc + 1], axis=0), bounds_check=p.breg(e, S - 1), oob_is_err=False),
                "xg%d" % gs, 16, reads=[b_idxi], writes=[b_xg[gs]])
            for k4 in range(4):
                tb = n["t"] % 2; n["t"] += 1
                for q in range(4):
                    k = k4 * 4 + q
                    p.op("tensor", lambda e, gs=gs, k=k, q=q, tb=tb: e.transpose(psT[tb][:, q * 128:(q + 1) * 128],
                                                                              xg[gs][:, k * 128:(k + 1) * 128], identb),
                         reads=[b_xg[gs], b_c2], writes=[b_psT[tb]], count=(q == 3))
                eng = "scalar" if n["ev"] % 2 == 0 else V
                n["ev"] += 1
                dstap = xinT[:, k4 * 4:k4 * 4 + 4, sc * 128:(sc + 1) * 128]
                srcap = psT[tb][:].rearrange("p (q f) -> p q f", q=4)
                if eng == "scalar":
                    p.op(eng, lambda e, dstap=dstap, srcap=srcap: e.copy(out=dstap, in_=srcap), reads=[b_psT[tb]], writes=[b_xin])
                else:
                    p.op(eng, lambda e, dstap=dstap, srcap=srcap: e.tensor_copy(out=dstap, in_=srcap), reads=[b_psT[tb]], writes=[b_xin])
        p.dma("gpsimd", wdt[:], wd[el].rearrange("(k p) n -> p k n", p=128), "wd", writes=[b_wd])
        wg_v = wg[el].rearrange("(k p) f -> p k f", p=128)
        wu_v = wu[el].rearrange("(k p) f -> p k f", p=128)
        for fc in range(8):
            ws = n["w"] % 2; n["w"] += 1
            p.dma("gpsimd", wgt[ws][:], wg_v[:, :, fc * 128:(fc + 1) * 128], "wg%d" % ws, writes=[b_wg[ws]])
            p.dma("gpsimd", wut[ws][:], wu_v[:, :, fc * 128:(fc + 1) * 128], "wu%d" % ws, writes=[b_wu[ws]])
            for hf in range(NSH):
                bg = (n["a"] % 2) * 2; n["a"] += 1
                for k in range(16):
                    p.op("tensor", lambda e, ws=ws, k=k, hf=hf, bg=bg: e.matmul(ps[bg][:], lhsT=wgt[ws][:, k, :],
                                                                               rhs=xinT[:, k, hf * 512:(hf + 1) * 512],
                                                                               start=(k == 0), stop=(k == 15)),
                         reads=[b_wg[ws], b_xin], writes=[b_ps[bg]], count=(k == 15))
                for k in range(16):
                    p.op("tensor", lambda e, ws=ws, k=k, hf=hf, bg=bg: e.matmul(ps[bg + 1][:], lhsT=wut[ws][:, k, :],
                                                                               rhs=xinT[:, k, hf * 512:(hf + 1) * 512],
                                                                               start=(k == 0), stop=(k == 15)),
                         reads=[b_wu[ws], b_xin], writes=[b_ps[bg + 1]], count=(k == 15))
                a = bg // 2
                p.op("scalar", lambda e, bg=bg, a=a: e.activation(out=act[a][:], in_=ps[bg][:], func=AF.Silu),
                     reads=[b_ps[bg]], writes=[b_act[a]])
                p.op(V, lambda e, bg=bg, a=a, fc=fc, hf=hf: e.tensor_tensor(out=hidT[:, fc, hf * 512:(hf + 1) * 512], in0=act[a][:],
                                                                          in1=ps[bg + 1][:], op=ALU.mult),
                     reads=[b_act[a], b_ps[bg + 1]], writes=[b_hid])
        for sc in range(CAP // 128):
            ys = n["y"] % 2; n["y"] += 1
            for dq in range(4):
                bk = 4 + (dq % 2)
                for fc in range(8):
                    p.op("tensor", lambda e, sc=sc, dq=dq, fc=fc, bk=bk: e.matmul(ps[bk][:], lhsT=hidT[:, fc, sc * 128:(sc + 1) * 128],
                                                                                 rhs=wdt[:, fc, dq * 512:(dq + 1) * 512],
                                                                                 start=(fc == 0), stop=(fc == 7)),
                         reads=[b_hid, b_wd], writes=[b_ps[bk]], count=(fc == 7))
                eng = "scalar" if dq % 2 == 0 else V
                if eng == "scalar":
                    p.op(eng, lambda e, ys=ys, dq=dq, bk=bk: e.copy(out=yb[ys][:, dq * 512:(dq + 1) * 512], in_=ps[bk][:]),
                         reads=[b_ps[bk]], writes=[b_yb[ys]])
                else:
                    p.op(eng, lambda e, ys=ys, dq=dq, bk=bk: e.tensor_copy(out=yb[ys][:, dq * 512:(dq + 1) * 512], in_=ps[bk][:]),
                         reads=[b_ps[bk]], writes=[b_yb[ys]])
            p.dma("sync", y_out[i, sc * 128:(sc + 1) * 128, :], yb[ys][:], "yo%d" % ys, reads=[b_yb[ys]])
    return p.finish()


def k4_consts(CAP=1024):
    UT = np.triu(np.ones((128, 128), np.float32))
    io = np.broadcast_to(np.arange(CAP, dtype=np.float32)[None, :], (128, CAP))
    return np.ascontiguousarray(np.concatenate([UT, io], axis=1))


def k4_consts2(NJ=64):
    ident = np.eye(128, dtype=np.float32)
    jp = np.zeros((128, NJ, 2), np.float32)
    jp[:, :, 0] = np.arange(NJ)[None, :]
    jp[:, :, 1] = np.arange(128)[:, None]
    return np.ascontiguousarray(np.concatenate([ident, jp.reshape(128, 2 * NJ)], axis=1))


def k4_inputs(c, aff, h_tm, z, L, S=8192):
    NJ = S // 128
    affp = np.zeros((128, 4, NJ), np.float32)
    for i in range(4):
        b, el = i // 2, i % 2
        affp[:, i, :] = aff[b * S:(b + 1) * S, 2 * c + el].reshape(NJ, 128).T
    return {"affp": affp.reshape(128, 4 * NJ), "h_tm0": h_tm[0:S], "h_tm1": h_tm[S:2 * S], "wg": z["w_gate"][L][2 * c:2 * c + 2], "wu": z["w_up"][L][2 * c:2 * c + 2],
            "wd": z["w_down"][L][2 * c:2 * c + 2], "cst": k4_consts(), "cst2": k4_consts2()}


D = 2048


def build_k5(T=2048, CAP=1024, NE=16):
    p = Prog()
    NTL = T // 128
    ys = [p.dram("y%d" % e, [CAP, D], BF16, "ExternalInput") for e in range(NE)]
    slot_tok = p.dram("slot_tok", [128, NTL * NE], F32, "ExternalInput")
    aff_tok = p.dram("aff_tok", [128, NTL * NE], F32, "ExternalInput")
    x1 = p.dram("x1", [T, D], F32, "ExternalInput")
    identd = p.dram("identd", [128, 128], F32, "ExternalInput")
    x2 = p.dram("x2", [T, D], F32, "ExternalOutput")

    sl = p.sb("sl", [128, NTL * NE], F32); b_sl = Buf("sl")
    gm = p.sb("gm", [128, NTL * NE], F32); b_gm = Buf("gm")
    sli = p.sb("sli", [128, NTL * NE], I32); b_sli = Buf("sli")
    idf = p.sb("idf", [128, 128], F32); b_id = Buf("id")
    NG = 6
    stg = [p.sb("stg%d" % i, [128, D], BF16) for i in range(NG)]; b_stg = [Buf("stg%d" % i) for i in range(NG)]
    dg = [p.sb("dg%d" % i, [128, 128], BF16) for i in range(4)]; b_dg = [Buf("dg%d" % i) for i in range(4)]
    acc = [p.sb("acc%d" % i, [128, D], F32) for i in range(2)]; b_acc = [Buf("acc0"), Buf("acc1")]
    ps = [p.ps("ps%d" % i, [128, 512]) for i in range(8)]; b_ps = [Buf("ps%d" % i) for i in range(8)]

    p.dma("sync", sl[:], slot_tok, "sl", writes=[b_sl])
    p.dma("sync", gm[:], aff_tok, "gm", writes=[b_gm])
    p.dma("sync", idf[:], identd, "id", writes=[b_id])
    msk = p.sb("msk", [128, NTL * NE], F32); b_msk = Buf("msk")
    p.op("vector", lambda e: e.tensor_scalar(out=msk[:], in0=sl[:], scalar1=0.0, scalar2=None, op0=ALU.is_ge), reads=[b_sl], writes=[b_msk])
    p.op("vector", lambda e: e.tensor_tensor(out=gm[:], in0=gm[:], in1=msk[:], op=ALU.mult), reads=[b_gm, b_msk], writes=[b_gm])
    p.op("vector", lambda e: e.tensor_scalar(out=msk[:], in0=msk[:], scalar1=-5000.0, scalar2=5000.0, op0=ALU.mult, op1=ALU.add),
         reads=[b_msk, b_gm], writes=[b_msk])
    p.op("vector", lambda e: e.tensor_tensor(out=sl[:], in0=sl[:], in1=msk[:], op=ALU.add), reads=[b_sl, b_msk], writes=[b_sl])
    p.op("vector", lambda e: e.tensor_copy(out=sli[:], in_=sl[:]), reads=[b_sl], writes=[b_sli])
    for i in range(NG):
        p.op("vector", lambda e, i=i: e.memset(stg[i][:], 0.0), writes=[b_stg[i]])
    n = {"g": 0, "d": 0}
    for tl in range(NTL):
        a = tl % 2
        pb = (tl % 2) * 4
        p.dma("sync", acc[a][:], x1[tl * 128:(tl + 1) * 128, :], "acc%d" % a, writes=[b_acc[a]])
        for e in range(NE):
            g = n["g"] % NG; n["g"] += 1
            d = n["d"] % 4; n["d"] += 1
            col = tl * NE + e
            p.raw("gpsimd", lambda e_, g=g, e=e, col=col: e_.indirect_dma_start(
                out=stg[g][:], out_offset=None, in_=ys[e][:, :],
                in_offset=bass.IndirectOffsetOnAxis(ap=sli[:, col:col + 1], axis=0), bounds_check=p.breg(e_, CAP - 1), oob_is_err=False),
                "stg%d" % g, 16, reads=[b_sli], writes=[b_stg[g]])
            p.op("vector", lambda e_, d=d, col=col: e_.tensor_scalar(out=dg[d][:], in0=idf[:], scalar1=gm[:, col:col + 1], scalar2=None,
                                                                  op0=ALU.mult), reads=[b_id, b_gm], writes=[b_dg[d]])
            for q in range(4):
                p.op("tensor", lambda e_, d=d, g=g, q=q, e=e, pb=pb: e_.matmul(ps[pb + q][:], lhsT=dg[d][:], rhs=stg[g][:, q * 512:(q + 1) * 512],
                                                                             start=(e == 0), stop=(e == NE - 1)),
                     reads=[b_dg[d], b_stg[g]], writes=[b_ps[pb + q]], count=(q == 3 or e == NE - 1))
        for q in range(4):
            p.op("vector", lambda e_, a=a, q=q, pb=pb: e_.tensor_tensor(out=acc[a][:, q * 512:(q + 1) * 512], in0=acc[a][:, q * 512:(q + 1) * 512],
                                                                       in1=ps[pb + q][:], op=ALU.add),
                 reads=[b_acc[a], b_ps[pb + q]], writes=[b_acc[a]])
        p.dma("sync", x2[tl * 128:(tl + 1) * 128, :], acc[a][:], "xo%d" % a, reads=[b_acc[a]])
    return p.finish()


def k5_inputs(c, Y, SL, aff, x1_tm):
    b = c // 4
    ins = {}
    slot_tok = np.zeros((128, 16, 16), np.float32)
    for e in range(16):
        ins["y%d" % e] = Y[e // 2][b * 2 + e % 2]
        slot_tok[:, :, e] = SL[e // 2][:, b * 2 + e % 2, (c % 4) * 16:(c % 4) * 16 + 16]
    aff_tok = np.ascontiguousarray(aff[c * 2048:(c + 1) * 2048].reshape(16, 128, 16).transpose(1, 0, 2))
    ins["slot_tok"] = slot_tok.reshape(128, 256)
    ins["aff_tok"] = aff_tok.reshape(128, 256)
    ins["x1"] = x1_tm[c * 2048:(c + 1) * 2048]
    ins["identd"] = np.eye(128, dtype=np.float32)
    return ins


_PROGS = {}


import time as _time
_T0 = [_time.time()]


def _lap(tag):
    t = _time.time()
    print("[lap] %s %.1fs" % (tag, t - _T0[0]), flush=True)
    _T0[0] = t


def _prog(name, fn):
    if name not in _PROGS:
        _PROGS[name] = fn()
    return _PROGS[name]


def kernel(**inp):
    z = {k: np.asarray(v) for k, v in inp.items()}
    NT = 16384
    x_tm = np.ascontiguousarray(z["x"].reshape(NT, 2048).astype(np.float32))
    for L in range(4):
        _lap('start layer')
        nw = np.ascontiguousarray(z["mix_norm_w"][L].reshape(16, 128).T)
        ins = [{"xT": np.ascontiguousarray(x_tm[c * 2048:(c + 1) * 2048].T), "nw": nw, "w_in": z["w_in"][L]} for c in range(8)]
        r = run(_prog("k1", build_k1), ins).results
        ufm = np.concatenate([r[c]["ufm"] for c in range(8)], axis=1)
        utm = np.concatenate([r[c]["utm"] for c in range(8)], axis=0)
        udt = np.concatenate([r[c]["udt"] for c in range(8)], axis=0)
        del r, ins
        _lap('before K2a')
        ins = [k2a_inputs(c // 4, c % 4, ufm, utm, z, L) for c in range(8)]
        r = run(_prog("k2a", build_k2a), ins).results
        o_na = np.zeros((NT, 512), NPBF)
        o_wa = np.zeros((NT, 512), NPBF)
        for c in range(8):
            b, j = c // 4, c % 4
            o_na[b * 8192:(b + 1) * 8192, j * 128:(j + 1) * 128] = r[c]["o_na"]
            o_wa[b * 8192:(b + 1) * 8192, j * 128:(j + 1) * 128] = r[c]["o_wa"]
        del r, ins
        _lap('before K2c')
        ins = [k2c_inputs(c // 4, c % 4, ufm, udt, z, L) for c in range(8)]
        r = run(_prog("k2c", build_k2c), ins).results
        y = np.zeros((NT, 512), np.float32)
        for c in range(8):
            b, j = c // 4, c % 4
            y[b * 8192:(b + 1) * 8192, j * 128:(j + 1) * 128] = r[c]["y_out"]
        del r, ins
        _lap('before K3')
        ins = [k3_inputs(c, x_tm, o_na, o_wa, y, ufm, z, L) for c in range(8)]
        r = run(_prog("k3", build_k3), ins).results
        x1_tm = np.concatenate([r[c]["x1T"].T for c in range(8)], axis=0)
        h_tm = np.ascontiguousarray(np.concatenate([r[c]["hT"].T for c in range(8)], axis=0))
        aff = np.concatenate([r[c]["aff"] for c in range(8)], axis=0)
        del r, ins, ufm, utm, udt
        _lap('before K4')
        ins = [k4_inputs(c, aff, h_tm, z, L) for c in range(8)]
        r = run(_prog("k4", build_k4), ins).results
        Y = [r[c]["y_out"] for c in range(8)]
        SL = [r[c]["slot_out"].reshape(128, 4, 64) for c in range(8)]
        del r, ins, h_tm
        _lap('before K5')
        ins = [k5_inputs(c, Y, SL, aff, x1_tm) for c in range(8)]
        r = run(_prog("k5", build_k5), ins).results
        x_tm = np.concatenate([r[c]["x2"] for c in range(8)], axis=0)
        del r, ins, Y, SL
        _lap('end layer')
        print("layer", L, "done; x std", float(x_tm.std()), "nan", int(np.isnan(x_tm).sum()), flush=True)
    return x_tm.reshape(2, 8192, 2048).astype(np.float32)
```
